# Optimizing a Trainium2 kernel written in Bass

```python
import jax, jax.numpy as jnp
from jax import lax
import numpy as np

D_MODEL = 1024
BATCH = 2
SEQ = 8192
DEPTH = 1

CHUNK = 64
D_BRANCH = D_MODEL // 2
D_CONV = D_BRANCH
CONV_WIDTH = 3
D_RWKV = D_BRANCH
RWKV_HEAD_DIM = 64
RWKV_HEADS = D_RWKV // RWKV_HEAD_DIM
DECAY_LORA = 64
AICL_LORA = 64
GATE_LORA = 128
MEM_LEN = 256
MEM_HEADS = 4
D_MEM = D_BRANCH
MEM_HEAD_DIM = D_MEM // MEM_HEADS
N_BRANCH = 3
D_IN_PROJ = 3 * D_CONV + 3 * D_RWKV + D_MEM
N_GROUPS = 8
EXPERTS_PER_GROUP = 8
N_EXPERTS = N_GROUPS * EXPERTS_PER_GROUP
TOP_K = 2
D_EXPERT = 512
ROUTE_BLOCK = 128
NORM_EPS = 1e-6
GN_EPS = 64e-5

kernel_name = "hybrid_conv_rwkv7_memxattn_hmoe_block"


def _rmsnorm(x, g):
    xf = x.astype(jnp.float32)
    y = xf * lax.rsqrt(jnp.mean(xf * xf, axis=-1, keepdims=True) + NORM_EPS)
    return (y * g.astype(jnp.float32)).astype(x.dtype)


def _shift(u):
    return jnp.pad(u, ((0, 0), (1, 0), (0, 0)))[:, :-1]


def _split_in_proj(p):
    sizes = [D_CONV] * 3 + [D_RWKV] * 3 + [D_MEM]
    offsets = np.cumsum(sizes)[:-1].tolist()
    return jnp.split(p, offsets, axis=-1)


def _short_conv_mixer(bg, cg, u, conv_w):
    seq = u.shape[1]
    cu = cg * u
    p = jnp.pad(cu, ((0, 0), (CONV_WIDTH - 1, 0), (0, 0)))
    conv = sum(p[:, j:j + seq] * conv_w[:, j] for j in range(CONV_WIDTH))
    return bg * conv


def _rwkv7_scan(r, w, k, v, kk, a):
    b, _, h, n = r.shape

    def step(state, inp):
        r_t, w_t, k_t, v_t, kk_t, a_t = inp
        sa = jnp.einsum('bhij,bhj->bhi', state, -kk_t)
        state = (state * w_t[:, :, None, :]
                 + sa[..., None] * (kk_t * a_t)[:, :, None, :]
                 + v_t[..., None] * k_t[:, :, None, :])
        y_t = jnp.einsum('bhij,bhj->bhi', state, r_t)
        return state, y_t

    xs = tuple(jnp.moveaxis(t, 1, 0) for t in (r, w, k, v, kk, a))
    s0 = jnp.zeros((b, h, n, n), jnp.float32)
    _, ys = lax.scan(step, s0, xs)
    return jnp.moveaxis(ys, 0, 1)


def _rwkv7_mixer(h, rp, kp, vp, mu_rkv, mu_wag, w_lora1, w_lora2, w0,
                 a_lora1, a_lora2, a0, g_lora1, g_lora2, k_k, k_a, r_k,
                 ln_x_w, ln_x_b):
    b, s, _ = h.shape
    r = rp + (_shift(rp) - rp) * mu_rkv[0]
    k = kp + (_shift(kp) - kp) * mu_rkv[1]
    v = vp + (_shift(vp) - vp) * mu_rkv[2]
    dh = _shift(h) - h
    xw = h + dh * mu_wag[0]
    xa = h + dh * mu_wag[1]
    xg = h + dh * mu_wag[2]
    w_log = -jax.nn.softplus(-(w0 + jnp.tanh(xw @ w_lora1) @ w_lora2)) - 0.5
    decay = jnp.exp(-jnp.exp(w_log.astype(jnp.float32)))
    a = jax.nn.sigmoid(a0 + (xa @ a_lora1) @ a_lora2)
    g = jax.nn.sigmoid(xg @ g_lora1) @ g_lora2

    def heads(t):
        return t.reshape(b, s, RWKV_HEADS, RWKV_HEAD_DIM).astype(jnp.float32)

    kk = heads(k * k_k)
    kk = kk * lax.rsqrt(jnp.maximum(jnp.sum(kk * kk, axis=-1, keepdims=True), 1e-24))
    k = k * (1 + (a - 1) * k_a)
    rh, kh, vh, ah, wh = heads(r), heads(k), heads(v), heads(a), heads(decay)
    y = _rwkv7_scan(rh, wh, kh, vh, kk, ah)
    mu = jnp.mean(y, axis=-1, keepdims=True)
    var = jnp.mean(jnp.square(y - mu), axis=-1, keepdims=True)
    y = (y - mu) * lax.rsqrt(var + GN_EPS)
    y = (y * ln_x_w.reshape(RWKV_HEADS, RWKV_HEAD_DIM).astype(jnp.float32)
         + ln_x_b.reshape(RWKV_HEADS, RWKV_HEAD_DIM).astype(jnp.float32))
    bonus = jnp.sum(rh * kh * r_k.astype(jnp.float32), axis=-1, keepdims=True) * vh
    return (y + bonus).reshape(b, s, D_RWKV).astype(h.dtype) * g


def _memory_attention(q, mem_n, w_kv_mem):
    b, s, _ = q.shape
    kv = mem_n @ w_kv_mem
    km, vm = jnp.split(kv, 2, axis=-1)
    qh = q.reshape(b, s, MEM_HEADS, MEM_HEAD_DIM).astype(jnp.float32)
    kh = km.reshape(b, -1, MEM_HEADS, MEM_HEAD_DIM).astype(jnp.float32)
    vh = vm.reshape(b, -1, MEM_HEADS, MEM_HEAD_DIM).astype(jnp.float32)
    scores = jnp.einsum('bshd,bmhd->bhsm', qh, kh) * (MEM_HEAD_DIM ** -0.5)
    p = jax.nn.softmax(scores, axis=-1)
    o = jnp.einsum('bhsm,bmhd->bshd', p, vh)
    return o.reshape(b, s, D_MEM).astype(q.dtype)


def _hierarchical_moe(h, w_router_group, b_router_group, w_router_expert,
                      b_router_expert, w_exp_gate, w_exp_up, w_exp_down):
    b, s, d = h.shape
    t = b * s
    ht = h.reshape(t, d)
    tok = jnp.arange(t)
    gp = jax.nn.softmax((ht @ w_router_group + b_router_group).astype(jnp.float32), axis=-1)
    g_sel = jnp.argmax(gp, axis=-1)
    g_w = jnp.max(gp, axis=-1, keepdims=True)
    elog = (ht @ w_router_expert + b_router_expert).astype(jnp.float32)
    elog = elog.reshape(t, N_GROUPS, EXPERTS_PER_GROUP)[tok, g_sel]
    ep = jax.nn.softmax(elog, axis=-1)
    top_p, top_i = lax.top_k(ep, TOP_K)
    comb = (g_w * top_p / jnp.sum(top_p, axis=-1, keepdims=True)).astype(h.dtype)
    expert_idx = g_sel[:, None] * EXPERTS_PER_GROUP + top_i

    n_assign = t * TOP_K
    e_flat = expert_idx.reshape(n_assign)
    tok_flat = jnp.arange(n_assign) // TOP_K
    order = jnp.argsort(e_flat)
    e_sorted = e_flat[order]
    tok_sorted = tok_flat[order]
    counts = jnp.bincount(e_flat, length=N_EXPERTS)
    starts = jnp.cumsum(counts) - counts
    padded = ((counts + ROUTE_BLOCK - 1) // ROUTE_BLOCK) * ROUTE_BLOCK
    pad_end = jnp.cumsum(padded)
    pad_start = pad_end - padded
    dest = pad_start[e_sorted] + (jnp.arange(n_assign) - starts[e_sorted])
    n_blocks = -(-n_assign // ROUTE_BLOCK) + N_EXPERTS
    buf = jnp.zeros((n_blocks * ROUTE_BLOCK, d), h.dtype).at[dest].set(ht[tok_sorted])
    blk_start = jnp.arange(n_blocks) * ROUTE_BLOCK
    blk_expert = jnp.minimum(jnp.searchsorted(pad_end, blk_start, side='right'), N_EXPERTS - 1)

    def run_block(args):
        xb, e = args
        hid = jax.nn.silu(xb @ w_exp_gate[e]) * (xb @ w_exp_up[e])
        return hid @ w_exp_down[e]

    yb = lax.map(run_block, (buf.reshape(n_blocks, ROUTE_BLOCK, d), blk_expert))
    y_rows = yb.reshape(n_blocks * ROUTE_BLOCK, d)[dest]
    w_sorted = comb.reshape(n_assign)[order]
    out = jax.ops.segment_sum(y_rows * w_sorted[:, None], tok_sorted, num_segments=t)
    return out.reshape(b, s, d)


def _hybrid_layer(x, mem, g_mix, g_mem, w_in, conv_w, mu_rkv, mu_wag, w_lora1,
                  w_lora2, w0, a_lora1, a_lora2, a0, g_lora1, g_lora2, k_k, k_a,
                  r_k, ln_x_w, ln_x_b, w_kv_mem, w_branch, w_gate, b_gate, w_o,
                  g_ffn, w_router_group, b_router_group, w_router_expert,
                  b_router_expert, w_exp_gate, w_exp_up, w_exp_down):
    b, s, _ = x.shape
    h = _rmsnorm(x, g_mix)
    bg, cg, u, rp, kp, vp, q = _split_in_proj(h @ w_in)
    y_conv = _short_conv_mixer(bg, cg, u, conv_w)
    y_rwkv = _rwkv7_mixer(h, rp, kp, vp, mu_rkv, mu_wag, w_lora1, w_lora2, w0,
                          a_lora1, a_lora2, a0, g_lora1, g_lora2, k_k, k_a, r_k,
                          ln_x_w, ln_x_b)
    y_mem = _memory_attention(q, _rmsnorm(mem, g_mem), w_kv_mem)
    branches = jnp.stack([y_conv, y_rwkv, y_mem], axis=2)
    proj = jnp.einsum('bsnc,ncd->bsnd', branches, w_branch)
    gates = jax.nn.sigmoid(h @ w_gate + b_gate).reshape(b, s, N_BRANCH, D_MODEL)
    z = jnp.sum(gates * proj, axis=2)
    x = x + z @ w_o
    h2 = _rmsnorm(x, g_ffn)
    return x + _hierarchical_moe(h2, w_router_group, b_router_group, w_router_expert,
                                 b_router_expert, w_exp_gate, w_exp_up, w_exp_down)


def setup_inputs(seed: int = 0) -> dict:
    key = jax.random.key(seed)
    ks = iter(jax.random.split(key, 40))

    def nrm(shape, scale):
        return jax.random.normal(next(ks), shape, jnp.float32) * scale

    def uni(shape, lo, hi):
        return jax.random.uniform(next(ks), shape, jnp.float32, lo, hi)

    L = DEPTH
    return {
        "x": nrm((BATCH, SEQ, D_MODEL), 1.0),
        "mem": nrm((BATCH, MEM_LEN, D_MODEL), 1.0),
        "g_mix": 1.0 + nrm((L, D_MODEL), 0.02),
        "g_mem": 1.0 + nrm((L, D_MODEL), 0.02),
        "w_in": nrm((L, D_MODEL, D_IN_PROJ), D_MODEL ** -0.5),
        "conv_w": nrm((L, D_CONV, CONV_WIDTH), CONV_WIDTH ** -0.5),
        "mu_rkv": uni((L, 3, D_RWKV), 0.0, 1.0),
        "mu_wag": uni((L, 3, D_MODEL), 0.0, 1.0),
        "w_lora1": nrm((L, D_MODEL, DECAY_LORA), D_MODEL ** -0.5),
        "w_lora2": nrm((L, DECAY_LORA, D_RWKV), DECAY_LORA ** -0.5),
        "w0": uni((L, D_RWKV), -2.0, 1.0),
        "a_lora1": nrm((L, D_MODEL, AICL_LORA), D_MODEL ** -0.5),
        "a_lora2": nrm((L, AICL_LORA, D_RWKV), AICL_LORA ** -0.5),
        "a0": uni((L, D_RWKV), -1.0, 1.0),
        "g_lora1": nrm((L, D_MODEL, GATE_LORA), D_MODEL ** -0.5),
        "g_lora2": nrm((L, GATE_LORA, D_RWKV), GATE_LORA ** -0.5),
        "k_k": 0.85 + nrm((L, D_RWKV), 0.05),
        "k_a": 1.0 + nrm((L, D_RWKV), 0.05),
        "r_k": nrm((L, RWKV_HEADS, RWKV_HEAD_DIM), 0.1),
        "ln_x_w": 1.0 + nrm((L, D_RWKV), 0.02),
        "ln_x_b": nrm((L, D_RWKV), 0.02),
        "w_kv_mem": nrm((L, D_MODEL, 2 * D_MEM), D_MODEL ** -0.5),
        "w_branch": nrm((L, N_BRANCH, D_BRANCH, D_MODEL), D_BRANCH ** -0.5),
        "w_gate": nrm((L, D_MODEL, N_BRANCH * D_MODEL), D_MODEL ** -0.5),
        "b_gate": nrm((L, N_BRANCH * D_MODEL), 0.02),
        "w_o": nrm((L, D_MODEL, D_MODEL), D_MODEL ** -0.5),
        "g_ffn": 1.0 + nrm((L, D_MODEL), 0.02),
        "w_router_group": nrm((L, D_MODEL, N_GROUPS), D_MODEL ** -0.5),
        "b_router_group": nrm((L, N_GROUPS), 0.01),
        "w_router_expert": nrm((L, D_MODEL, N_EXPERTS), D_MODEL ** -0.5),
        "b_router_expert": nrm((L, N_EXPERTS), 0.01),
        "w_exp_gate": nrm((L, N_EXPERTS, D_MODEL, D_EXPERT), D_MODEL ** -0.5),
        "w_exp_up": nrm((L, N_EXPERTS, D_MODEL, D_EXPERT), D_MODEL ** -0.5),
        "w_exp_down": nrm((L, N_EXPERTS, D_EXPERT, D_MODEL), D_EXPERT ** -0.5),
        "g_final": 1.0 + nrm((D_MODEL,), 0.02),
    }


def reference(x, mem, g_mix, g_mem, w_in, conv_w, mu_rkv, mu_wag, w_lora1, w_lora2,
              w0, a_lora1, a_lora2, a0, g_lora1, g_lora2, k_k, k_a, r_k, ln_x_w,
              ln_x_b, w_kv_mem, w_branch, w_gate, b_gate, w_o, g_ffn,
              w_router_group, b_router_group, w_router_expert, b_router_expert,
              w_exp_gate, w_exp_up, w_exp_down, g_final):
    for l in range(DEPTH):
        x = _hybrid_layer(x, mem, g_mix[l], g_mem[l], w_in[l], conv_w[l], mu_rkv[l],
                          mu_wag[l], w_lora1[l], w_lora2[l], w0[l], a_lora1[l],
                          a_lora2[l], a0[l], g_lora1[l], g_lora2[l], k_k[l], k_a[l],
                          r_k[l], ln_x_w[l], ln_x_b[l], w_kv_mem[l], w_branch[l],
                          w_gate[l], b_gate[l], w_o[l], g_ffn[l], w_router_group[l],
                          b_router_group[l], w_router_expert[l], b_router_expert[l],
                          w_exp_gate[l], w_exp_up[l], w_exp_down[l])
    return _rmsnorm(x, g_final)
```

```python
import numpy as np
from contextlib import ExitStack
import concourse.bass as bass
import concourse.mybir as mybir
from concourse.bass_utils import run_bass_kernel_spmd

F32 = mybir.dt.float32
BF16 = mybir.dt.bfloat16
AF = mybir.ActivationFunctionType
ALU = mybir.AluOpType
AX = mybir.AxisListType

NCORES = 8
DEV = {}
TOK = 2048
WIN = 8192
BT = 512
NBLK = WIN // BT
OWN0 = NBLK - TOK // BT
CDEC = 0.6065306597126334
GCAP = 384

C_ID, C_BO, C_M3, C_RS, C_ONE, C_LT, C_IOTA = 0, 128, 256, 640, 1152, 1280, 1408
NCON = C_IOTA + GCAP
PV = {}
_o = 0
for _n, _w in [("g_mix", 8), ("g_mem", 8), ("g_ffn", 8), ("mu_w", 8), ("mu_a", 8), ("mu_g", 8), ("b_gate", 24),
               ("conv_w", 12), ("k_k", 4), ("k_a", 4), ("w0", 4), ("a0", 4), ("ln_w", 4), ("ln_b", 4), ("r_k", 4)]:
    PV[_n] = _o
    _o += _w
NPV = _o
BV_MU, BV_GF, BV_RB, BV_GN = 0, 1536, 2560, 2632
NBV = 2632 + 1024


class FW:
    EPOCH = 2000
    NDMA = 8

    def __init__(self, nc):
        self.nc = nc
        self.ops = []
        self.opmap = {}
        self.last_write = {}
        self.readers = {}
        self.engs = ['pe', 'act', 'dve', 'pool', 'sp']
        self.sems = {e: [] for e in self.engs}
        self.count = {e: 0 for e in self.engs}
        self.dma_sems = {}
        self.dma_n = {e: 0 for e in self.engs}
        self.dma_last = {}
        self.waited = {e: {} for e in self.engs}
        self.stack = ExitStack()
        self.barrier_tokens = []
        self.nid = 0
        self.n_inst = 0

    def _sem(self, name):
        return self.stack.enter_context(self.nc.semaphore(name))

    def op(self, eng, fn, reads=(), writes=(), dma=False):
        deps = set()
        for r in reads:
            if r in self.last_write:
                deps.add(self.last_write[r])
        for w in writes:
            if w in self.last_write:
                deps.add(self.last_write[w])
            for x in self.readers.get(w, ()):
                deps.add(x)
        oid = self.nid
        self.nid += 1
        o = dict(id=oid, eng=eng, fn=fn, deps=deps, dma=dma, sig=dma, token=None)
        if dma:
            slot = (eng, self.dma_n[eng] % self.NDMA)
            self.dma_n[eng] += 1
            if slot in self.dma_last:
                deps.add(self.dma_last[slot])
            self.dma_last[slot] = oid
            o['slot'] = slot
        if eng == 'pe' and not dma:
            o['deps'] = set(d for d in deps if not (d in self.opmap and self.opmap[d]['eng'] == 'pe' and not self.opmap[d]['dma']))
        self.ops.append(o)
        self.opmap[oid] = o
        for r in reads:
            self.readers.setdefault(r, []).append(oid)
        for w in writes:
            self.last_write[w] = oid
            self.readers[w] = []
        return oid

    def flush(self, final=False):
        nc = self.nc
        ops = self.ops
        ids = set(o['id'] for o in ops)
        for o in ops:
            o['deps'] = set(d for d in o['deps'] if d in ids)
            for d in o['deps']:
                self.opmap[d]['sig'] = True
        last = {}
        for o in ops:
            if not o['dma']:
                last[o['eng']] = o
        for o in last.values():
            o['sig'] = True
        for o in ops:
            if o['dma']:
                key = o['slot']
                if key not in self.dma_sems:
                    self.dma_sems[key] = [self._sem(f"dma_{key[0]}_{key[1]}"), 0]
                ent = self.dma_sems[key]
                ent[1] += 16
                o['token'] = (ent[0], ent[1], f"dma_{key[0]}_{key[1]}")
            elif o['sig']:
                e = o['eng']
                c = self.count[e]
                ep = c // self.EPOCH
                while len(self.sems[e]) <= ep:
                    self.sems[e].append(self._sem(f"s_{e}_{len(self.sems[e])}"))
                o['token'] = (self.sems[e][ep], c % self.EPOCH + 1, f"s_{e}_{ep}")
                self.count[e] = c + 1
        per_eng = {e: [o for o in ops if o['eng'] == e] for e in self.engs}
        start_tokens = list(self.barrier_tokens)
        end_tokens = [o['token'] for o in last.values()] + [o['token'] for o in ops if o['dma']]

        def emit(e, eng):
            waited = self.waited[e]

            def wait(tok):
                sem, val, name = tok
                if waited.get(name, 0) >= val:
                    return
                eng.wait_ge(sem, val)
                self.n_inst += 1
                waited[name] = val
            for t in start_tokens:
                wait(t)
            for o in per_eng[e]:
                for d in sorted(o['deps']):
                    wait(self.opmap[d]['token'])
                ins = o['fn'](eng)
                self.n_inst += 1
                if o['token'] is not None:
                    ins.then_inc(o['token'][0], 16 if o['dma'] else 1)
            if final and e == 'sp':
                for t in end_tokens:
                    wait(t)

        with nc.Block() as block:
            @block.tensor
            def _(eng):
                emit('pe', eng)

            @block.scalar
            def _(eng):
                emit('act', eng)

            @block.vector
            def _(eng):
                emit('dve', eng)

            @block.gpsimd
            def _(eng):
                emit('pool', eng)

            @block.sync
            def _(eng):
                emit('sp', eng)
        self.barrier_tokens = end_tokens
        for o in ops:
            o['fn'] = None
        self.ops = []
        self.opmap = {}
        self.last_write = {}
        self.readers = {}
        self.dma_last = {}


class K:
    def __init__(self, nc):
        self.nc = nc
        self.fw = FW(nc)
        self.uid = 0

    def sb(self, st, name, shape, dt):
        self.uid += 1
        return st.enter_context(self.nc.sbuf_tensor(f"{name}_{self.uid}", shape, dt))

    def ps(self, st, name, shape, dt=F32):
        self.uid += 1
        return st.enter_context(self.nc.psum_tensor(f"{name}_{self.uid}", shape, dt))

    def mm(self, out, lhsT, rhs, start=True, stop=True, r=(), w=(), tp=None):
        if tp is None:
            self.fw.op('pe', lambda e: e.matmul(out, lhsT=lhsT, rhs=rhs, start=start, stop=stop), r, w)
        else:
            self.fw.op('pe', lambda e: e.matmul(out, lhsT=lhsT, rhs=rhs, start=start, stop=stop, tile_position=tp), r, w)

    def tr(self, out, in_, ident, r=(), w=()):
        self.fw.op('pe', lambda e: e.transpose(out, in_, ident), r, w)

    def act(self, out, in_, func, r=(), w=(), **kw):
        self.fw.op('act', lambda e: e.activation(out=out, in_=in_, func=func, **kw), r, w)

    def tt(self, eng, out, in0, in1, op, r=(), w=()):
        self.fw.op(eng, lambda e: e.tensor_tensor(out=out, in0=in0, in1=in1, op=op), r, w)

    def ts(self, eng, out, in0, s1, op0, s2=None, op1=None, r=(), w=()):
        if op1 is None:
            self.fw.op(eng, lambda e: e.tensor_scalar(out=out, in0=in0, scalar1=s1, scalar2=None, op0=op0), r, w)
        else:
            self.fw.op(eng, lambda e: e.tensor_scalar(out=out, in0=in0, scalar1=s1, scalar2=s2, op0=op0, op1=op1), r, w)

    def stt(self, out, in0, scalar, in1, op0, op1, r=(), w=()):
        self.fw.op('dve', lambda e: e.scalar_tensor_tensor(out=out, in0=in0, scalar=scalar, in1=in1, op0=op0, op1=op1), r, w)

    def cp(self, eng, out, in_, r=(), w=()):
        if eng == 'act':
            self.act(out, in_, AF.Copy, r, w)
        else:
            self.fw.op(eng, lambda e: e.tensor_copy(out=out, in_=in_), r, w)

    def memset(self, eng, ap, val, w=()):
        self.fw.op(eng, lambda e: e.memset(ap, val), (), w)

    def recip(self, out, in_, r=(), w=()):
        self.fw.op('dve', lambda e: e.reciprocal(out=out, in_=in_), r, w)

    def dma(self, eng, out, in_, r=(), w=()):
        self.fw.op(eng, lambda e: e.dma_start(out=out, in_=in_), r, w, dma=True)


def run_interleaved(gens):
    gens = [g for g in gens if g is not None]
    while gens:
        for g in list(gens):
            try:
                next(g)
            except StopIteration:
                gens.remove(g)


def build_program(dbg=None):
    nc = bass.Bass("TRN2", target_bir_lowering=False)
    k = K(nc)
    fw = k.fw

    def din(name, shape):
        return nc.dram_tensor(name, shape, F32, kind="ExternalInput").ap()

    xext = din("xext", [WIN, 1024])
    mem = din("mem", [256, 1024])
    w_in = din("w_in", [1024, 3584])
    w_gate = din("w_gate", [1024, 3072])
    w_branch = din("w_branch", [1536, 1024])
    w_o = din("w_o", [1024, 1024])
    w_kv = din("w_kv", [1024, 1024])
    l1 = din("l1", [1024, 256])
    l2wa = din("l2wa", [128, 512])
    l2g = din("l2g", [128, 512])
    wr = din("wr", [1024, 72])
    if dbg is None or dbg == 'C':
        weg = din("weg", [64, 1024, 512])
        weu = din("weu", [64, 1024, 512])
        wed = din("wed", [64, 512, 1024])
    pvec = din("pvec", [128, NPV])
    bvec = din("bvec", [128, NBV])
    consts = din("consts", [128, NCON])
    out = nc.dram_tensor("out", [TOK, 1024], F32, kind="ExternalOutput").ap()
    x1s = nc.dram_tensor("x1s", [TOK, 1024], F32, kind="Internal").ap()
    dbg_ap = None
    if dbg == 'A':
        dbg_ap = nc.dram_tensor("dbg", [512, TOK], F32, kind="ExternalOutput").ap()
    if dbg == 'B':
        dbg_ap = nc.dram_tensor("dbg", [TOK, 1024], F32, kind="ExternalOutput").ap()
        dbg2_ap = nc.dram_tensor("dbg2", [128, 16 * 72], F32, kind="ExternalOutput").ap()

    G = ExitStack()
    cb = k.sb(G, "cb", [128, NCON], BF16)
    pv = k.sb(G, "pv", [128, NPV], F32)
    pvd = k.sb(G, "pvd", [128, 40], F32)
    yrw = k.sb(G, "yrw", [128, 4, TOK], BF16)
    k.dma('pool', cb[:], consts, w=['cb'])
    k.dma('sp', pv[:], pvec, w=['pv'])
    ident = cb[:, C_ID:C_ID + 128]
    bones = cb[:, C_BO:C_BO + 128]
    mask3 = cb[:, C_M3:C_M3 + 384]
    mask2 = cb[:, C_M3:C_M3 + 256]
    rsmask = cb[:, C_RS:C_RS + 512]
    ones_b = cb[:, C_ONE:C_ONE + 128]
    lt_b = cb[:, C_LT:C_LT + 128]
    iota_b = cb[:, C_IOTA:C_IOTA + GCAP]

    def pvc(name, i=0, n=1):
        return pv[:, PV[name] + i:PV[name] + i + n]

    for i, nm in enumerate(["mu_w", "mu_a", "mu_g"]):
        k.ts('dve', pvd[:, i * 8:(i + 1) * 8], pvc(nm, 0, 8), -1.0, ALU.mult, 1.0, ALU.add, r=['pv'], w=[('pvd', i)])
    k.ts('dve', pvd[:, 24:28], pvc("k_a", 0, 4), -1.0, ALU.mult, 1.0, ALU.add, r=['pv'], w=[('pvd', 3)])
    k.ts('dve', pvd[:, 28:32], pvc("a0", 0, 4), 0.5, ALU.mult, r=['pv'], w=[('pvd', 4)])
    k.ts('dve', pvd[:, 32:36], pvc("w0", 0, 4), 0.5, ALU.mult, r=['pv'], w=[('pvd', 5)])
    PVD_ALL = [('pvd', i) for i in range(6)]

    PS = [k.ps(G, f"bank{i}", [128, 512]) for i in range(4)]
    PD = [k.ps(G, f"pd{i}", [128, 512]) for i in range(4)]
    PENG = ['act', 'act', 'dve', 'dve']

    A = ExitStack()
    W1 = k.sb(A, "W1", [128, 8, 1536], BF16)
    W2 = k.sb(A, "W2", [128, 8, 1536], BF16)
    L1a = k.sb(A, "L1a", [128, 8, 256], BF16)
    L1b = k.sb(A, "L1b", [128, 8, 256], BF16)
    L2wa = k.sb(A, "L2wa", [128, 512], BF16)
    L2g = k.sb(A, "L2g", [128, 512], BF16)
    RKbd = k.sb(A, "RKbd", [128, 4, 128], BF16)
    k.dma('pool', L2wa[:], l2wa, w=['L2wa'])
    k.dma('pool', L2g[:], l2g, w=['L2g'])
    for p in range(4):
        k.ts('dve', RKbd[:, p, :], bones, pvc("r_k", p), ALU.mult, r=['cb', 'pv'], w=[('RKbd', p)])

    with ExitStack() as Wp:
        mub = k.sb(Wp, "mub", [128, 1536], F32)
        omub = k.sb(Wp, "omub", [128, 1536], F32)
        stg = [k.sb(Wp, f"stg{i}", [128, 1536], F32) for i in range(2)]
        l1s = k.sb(Wp, "l1s", [128, 8, 256], F32)
        k.dma('sp', mub[:], bvec[:, BV_MU:BV_MU + 1536], w=['mub'])
        k.ts('dve', omub[:], mub[:], -1.0, ALU.mult, 1.0, ALU.add, r=['mub'], w=['omub'])
        k.dma('sp', l1s[:], l1.rearrange("(k p) n -> p k n", p=128), w=['l1s'])
        for kc in range(8):
            s = stg[kc % 2]
            k.dma('sp', s[:], w_in[kc * 128:(kc + 1) * 128, 1536:3072], w=[('stg', kc % 2)])
            gk = pvc("g_mix", kc)
            k.stt(W1[:, kc, :], s[:], gk, omub[:], ALU.mult, ALU.mult, r=[('stg', kc % 2), 'omub', 'pv'], w=[('W1', kc)])
            k.stt(W2[:, kc, :], s[:], gk, mub[:], ALU.mult, ALU.mult, r=[('stg', kc % 2), 'mub', 'pv'], w=[('W2', kc)])
            for gi, (c0, c1) in enumerate([(0, 64), (64, 128), (128, 256)]):
                k.ts('dve', L1a[:, kc, c0:c1], l1s[:, kc, c0:c1], gk, ALU.mult, pvd[:, gi * 8 + kc:gi * 8 + kc + 1], ALU.mult,
                     r=['l1s', 'pv'] + PVD_ALL, w=[('L1a', kc, gi)])
                k.ts('dve', L1b[:, kc, c0:c1], l1s[:, kc, c0:c1], gk, ALU.mult, pv[:, PV["mu_w"] + gi * 8 + kc:PV["mu_w"] + gi * 8 + kc + 1], ALU.mult,
                     r=['l1s', 'pv'], w=[('L1b', kc, gi)])
        fw.flush()
    WKEYS = [('W1', kc) for kc in range(8)] + [('W2', kc) for kc in range(8)]
    LKEYS = [('L1a', kc, gi) for kc in range(8) for gi in range(3)] + [('L1b', kc, gi) for kc in range(8) for gi in range(3)]

    hT = [k.sb(A, "hT0", [128, 8, BT + 1], BF16)] * 2
    xt = [k.sb(A, "xt0", [128, 1024], BF16)] * 2
    xn = k.sb(A, "xn", [128, 1024], BF16)
    junk = xn
    stat = k.sb(A, "stat", [128, 8], F32)
    mhalf = k.sb(A, "mhalf", [128, 8], F32)
    k.memset('pool', mhalf[:], -0.5, w=['mhalf'])
    lwa = k.sb(A, "lwa", [128, BT], BF16)
    lg = k.sb(A, "lg", [128, BT], BF16)
    TMPN = ["kk", "rn", "a", "t", "kmod", "sig", "cum", "cp", "Wa", "Wb", "r32"]
    TBF = ("kk", "a", "t", "kmod", "Wa", "Wb", "r32")
    TS = [{n: k.sb(A, f"t{i}_" + n, [128, BT], BF16 if n in TBF else F32) for n in TMPN} for i in range(2)]
    SQ = [k.sb(A, "sq0", [128, BT], BF16)] * 2
    tmp = TS[0]
    sq = SQ[0]
    AR = [[k.sb(A, f"AR{p}{q}", [128, 2, BT], BF16) for q in range(2)] for p in range(4)]
    BK = [[k.sb(A, f"BK{p}{q}", [128, 2, BT], BF16) for q in range(2)] for p in range(4)]
    VB = [[k.sb(A, f"VB{p}{q}", [128, BT], BF16) for q in range(2)] for p in range(4)]
    WC = [[k.sb(A, f"WC{p}{q}", [128, 8], F32) for q in range(2)] for p in range(4)]
    GB = [[k.sb(A, f"GB{p}{q}", [128, BT], BF16) for q in range(2)] for p in range(4)]
    Yb = [k.sb(A, f"Yb{p}", [128, BT], BF16) for p in range(4)]
    NS = 8
    PM = [k.sb(A, f"PM{s}", [128, 384], BF16) for s in range(NS)]
    MK = [k.sb(A, f"MK{s}", [128, 256], BF16) for s in range(NS)]
    TK = [k.sb(A, f"TK{s}", [128, 192], BF16) for s in range(NS)]
    TTf = [k.sb(A, f"TTf{s}", [128, 128], BF16) for s in range(NS)]
    TT1 = TTf
    DD = [[k.sb(A, f"DD{s}{i}", [128, 384], BF16) for i in range(2)] for s in range(4)]
    S32 = [k.sb(A, f"S32{p}", [128, 64], F32) for p in range(4)]
    Sbf = [k.sb(A, f"Sbf{p}", [128, 64], BF16) for p in range(4)]
    Xb = [k.sb(A, f"Xb{p}", [128, 64], BF16) for p in range(4)]
    Ub = [k.sb(A, f"Ub{p}", [128, 64], BF16) for p in range(4)]
    Stmp = [k.sb(A, f"Stmp{p}", [128, 64], F32) for p in range(4)]
    for p in range(4):
        k.memset('pool', S32[p][:], 0.0, w=[('S32', p)])
        k.memset('pool', Sbf[p][:], 0.0, w=[('Sbf', p)])
    for b in (1, 2):
        k.memset('dve', PS[b][:, :], 0.0, w=[('psprep', b - 1)])

    def rmsnorm_T(src_rows, xt_i, dst, dst_key, gcols=None, f32dst=None):
        t = xt[xt_i]
        k.dma('pool', t[:], src_rows, w=[('xt', xt_i)])
        yield
        k.act(junk[:], t[:], AF.Square, r=[('xt', xt_i)], w=['xn', ('stat', 0)], accum_out=stat[:, 0:1])
        yield
        k.ts('dve', stat[:, 1:2], stat[:, 0:1], 1.0 / 1024, ALU.mult, 1e-6, ALU.add, r=[('stat', 0)], w=[('stat', 1)])
        yield
        k.tt('pool', stat[:, 3:4], stat[:, 1:2], mhalf[:, 0:1], ALU.pow, r=[('stat', 1), 'mhalf'], w=[('stat', 3)])
        yield
        k.act(xn[:], t[:], AF.Copy, r=[('xt', xt_i), ('stat', 3)], w=['xn'], scale=stat[:, 3:4])
        yield
        psb = PS[0][:, :].bitcast(BF16)
        for kc in range(8):
            k.tr(psb[:, kc * 128:(kc + 1) * 128], xn[:, kc * 128:(kc + 1) * 128], ident, r=['xn', 'cb'], w=[('ps', 0)])
        k.cp('act', dst, psb[:, 0:1024].rearrange("p (k t) -> p k t", k=8), r=[('ps', 0)], w=[dst_key])
        yield

    CH = [(p // 2, (p % 2) * 192) for p in range(4)]

    def PDK(p, what):
        return ('pd', p)

    def chain_regions(p):
        return PD[p][:, 384:448], PD[p][:, 448:512]

    def frontend(blk):
        own = blk >= OWN0
        q = blk % 2
        par = 0
        hb = hT[0]
        if blk == 0:
            k.memset('pool', hb[:, :, 0:1], 0.0, w=[('hT', par, 'c0')])
        else:
            k.cp('pool', hb[:, :, 0:1], hb[:, :, BT:BT + 1], r=[('hT', par, 3)], w=[('hT', par, 'c0')])
        for tt_ in range(4):
            row0 = blk * BT + tt_ * 128
            for _ in rmsnorm_T(xext[row0:row0 + 128, :], 0, hb[:, :, 1 + tt_ * 128:1 + (tt_ + 1) * 128], ('hT', par, tt_)):
                yield
        HK = [('hT', par, i) for i in range(4)] + [('hT', par, 'c0')]
        for m in range(2 if own else 1):
            for kc in range(8):
                k.mm(PS[0][:, :], L1a[:, kc, m * 128:(m + 1) * 128], hb[:, kc, 1:BT + 1], start=(kc == 0), stop=False, r=HK + LKEYS, w=[('ps', 0)])
            for kc in range(8):
                k.mm(PS[0][:, :], L1b[:, kc, m * 128:(m + 1) * 128], hb[:, kc, 0:BT], start=False, stop=(kc == 7), r=HK + LKEYS, w=[('ps', 0)])
            yield
            if m == 0:
                k.act(lwa[0:64, :], PS[0][0:64, :], AF.Tanh, r=[('ps', 0)], w=[('lwa', 0)])
                k.act(lwa[64:128, :], PS[0][64:128, :], AF.Copy, r=[('ps', 0)], w=[('lwa', 1)])
            else:
                k.act(lg[:], PS[0][:, :], AF.Sigmoid, r=[('ps', 0)], w=['lg'])
            yield

        def part1(p):
            si = p % 2
            T = TS[si]
            K_ = lambda n: ('t', si, n)
            pc = slice(p * 128, (p + 1) * 128)
            k.mm(PS[0][:, :], L2wa[64:128, pc], lwa[64:128, :], r=['L2wa', ('lwa', 1)], w=[('ps', 0)])
            k.act(T["a"][:], PS[0][:, :], AF.Tanh, r=[('ps', 0)] + PVD_ALL, w=[K_('a')], bias=pvd[:, 28 + p:29 + p], scale=0.5)
            yield
            k.ts('dve', T["a"][:], T["a"][:], 0.5, ALU.mult, 0.5, ALU.add, r=[K_('a')], w=[K_('a')])
            yield
            k.mm(PS[0][:, :], L2wa[0:64, pc], lwa[0:64, :], r=['L2wa', ('lwa', 0)], w=[('ps', 0)])
            k.act(T["sig"][:], PS[0][:, :], AF.Tanh, r=[('ps', 0)] + PVD_ALL, w=[K_('sig')], bias=pvd[:, 32 + p:33 + p], scale=0.5)
            yield
            k.ts('dve', T["sig"][:], T["sig"][:], 0.5, ALU.mult, 0.5, ALU.add, r=[K_('sig')], w=[K_('sig')])
            yield
            if own:
                k.mm(PS[0][:, :], L2g[:, pc], lg[:], r=['L2g', 'lg'], w=[('ps', 0)])
                k.act(GB[p][q][:], PS[0][:, :], AF.Copy, r=[('ps', 0)], w=[('GB', p, q)])
                yield
            yield
            k.ts('pool', T["t"][:], T["a"][:], pvc("k_a", p), ALU.mult, pvd[:, 24 + p:25 + p], ALU.add, r=[K_('a'), 'pv'] + PVD_ALL, w=[K_('t')])
            yield
            for qi, bank in ((1, 3), (2, 0), (0, 0)):
                if qi == 0 and not own:
                    continue
                for kc in range(8):
                    k.mm(PS[bank][:, :], W1[:, kc, qi * 512 + p * 128:qi * 512 + (p + 1) * 128], hb[:, kc, 1:BT + 1], start=(kc == 0), stop=False, r=HK + WKEYS, w=[('ps', bank)])
                for kc in range(8):
                    k.mm(PS[bank][:, :], W2[:, kc, qi * 512 + p * 128:qi * 512 + (p + 1) * 128], hb[:, kc, 0:BT], start=False, stop=(kc == 7), r=HK + WKEYS, w=[('ps', bank)])
                if qi == 1:
                    k.ts('dve', T["kk"][:], PS[bank][:, :], pvc("k_k", p), ALU.mult, r=[('ps', bank), 'pv'], w=[K_('kk')])
                    yield
                    k.tt('dve', T["kmod"][:], PS[bank][:, :], T["t"][:], ALU.mult, r=[('ps', bank), K_('t')], w=[K_('kmod')])
                    yield
                elif qi == 2:
                    k.cp('act', VB[p][q][:], PS[bank][:, :], r=[('ps', bank)], w=[('VB', p, q)])
                    yield
                else:
                    k.cp('act', T["r32"][:], PS[bank][:, :], r=[('ps', bank)], w=[K_('r32')])
                    yield
                yield

        def part2(p):
            si = p % 2
            T = TS[si]
            K_ = lambda n: ('t', si, n)
            sq_ = SQ[si]
            k.act(sq_[:], T["kk"][:], AF.Square, r=[K_('kk')], w=[('sq', 0)])
            yield
            k.mm(PS[3][:, :], bones, sq_[:], r=['cb', ('sq', 0)], w=[('ps', 3)])
            k.ts('dve', T["rn"][:], PS[3][:, :], 1e-24, ALU.max, r=[('ps', 3)], w=[K_('rn')])
            yield
            k.act(T["rn"][:], T["rn"][:], AF.Sqrt, r=[K_('rn')], w=[K_('rn')])
            yield
            k.recip(T["rn"][:], T["rn"][:], r=[K_('rn')], w=[K_('rn')])
            yield
            k.tt('pool', T["kk"][:], T["kk"][:], T["rn"][:], ALU.mult, r=[K_('kk'), K_('rn')], w=[K_('kk')])
            yield
            yield
            fw.op('dve', lambda e: e.tensor_tensor_scan(out=T["cum"][:], data0=rsmask, data1=T["sig"][:], initial=0.0, op0=ALU.mult, op1=ALU.add),
                  ['cb', K_('sig')], [K_('cum')])
            k.tt('pool', T["cp"][:], T["cum"][:], T["sig"][:], ALU.subtract, r=[K_('cum'), K_('sig')], w=[K_('cp')])
            yield
            cumv = T["cum"][:, :].rearrange("p (c t) -> p c t", t=64)
            k.act(WC[p][q][:, :], cumv[:, :, 63], AF.Exp, r=[K_('cum')], w=[('WC', p, q)], scale=-CDEC)
            yield
            k.act(T["Wa"][:], T["cp"][:], AF.Exp, r=[K_('cp')], w=[K_('Wa')], scale=-CDEC)
            yield
            k.stt(AR[p][q][:, 0, :], T["kk"][:], -1.0, T["Wa"][:], ALU.mult, ALU.mult, r=[K_('kk'), K_('Wa')], w=[('AR', p, q, 0)])
            yield
            yield
            k.act(T["Wb"][:], T["cum"][:], AF.Exp, r=[K_('cum')], w=[K_('Wb')], scale=CDEC)
            yield
            k.tt('pool', T["t"][:], T["kk"][:], T["a"][:], ALU.mult, r=[K_('kk'), K_('a'), K_('t')], w=[K_('t')])
            yield
            k.tt('dve', BK[p][q][:, 0, :], T["t"][:], T["Wb"][:], ALU.mult, r=[K_('t'), K_('Wb')], w=[('BK', p, q, 0)])
            yield
            k.tt('dve', BK[p][q][:, 1, :], T["kmod"][:], T["Wb"][:], ALU.mult, r=[K_('kmod'), K_('Wb')], w=[('BK', p, q, 1)])
            yield
            if own:
                k.act(T["Wa"][:], T["cum"][:], AF.Exp, r=[K_('cum'), K_('Wa')], w=[K_('Wa')], scale=-CDEC)
                yield
                k.tt('dve', AR[p][q][:, 1, :], T["r32"][:], T["Wa"][:], ALU.mult, r=[K_('r32'), K_('Wa')], w=[('AR', p, q, 1)])
                yield
                t0 = (blk - OWN0) * BT
                k.tt('pool', sq_[:], T["r32"][:], T["kmod"][:], ALU.mult, r=[K_('r32'), K_('kmod'), ('sq', 0)], w=[('sq', 0)])
                yield
                k.mm(PS[3][:, :], RKbd[:, p, :], sq_[:], r=[('RKbd', p), ('sq', 0)], w=[('ps', 3)])
                k.tt('dve', T["cp"][:], PS[3][:, :], VB[p][q][:], ALU.mult, r=[('ps', 3), ('VB', p, q)], w=[K_('cp')])
                yield
                k.tt('dve', yrw[:, p, t0:t0 + BT], T["cp"][:], GB[p][q][:], ALU.mult, r=[K_('cp'), ('GB', p, q)], w=[('yrw', p, blk - OWN0)])
                yield
            yield

        for _ in part1(0):
            yield
        for p_ in range(4):
            gens = [part2(p_)] + ([part1(p_ + 1)] if p_ + 1 < 4 else [])
            while gens:
                for g_ in list(gens):
                    if next(g_, 'done') == 'done':
                        gens.remove(g_)
                yield

    def prep_pass(blk, c, p, pss):
        own = blk >= OWN0
        q = blk % 2
        tc = slice(c * 64, (c + 1) * 64)
        sl0 = (c % 2) * 4
        if True:
            s = sl0 + p
            pb = p % 2
            bank = PS[1 + pb]
            psM = bank[:, 0:384].rearrange("p (g h t) -> p g h t", g=3, h=2)
            psKT = bank[:, 384:512]
            ARt, BKt, VBt = AR[p][q], BK[p][q], VB[p][q]
            rk_ar = [('AR', p, q, 0)] + ([('AR', p, q, 1)] if own else [])
            rk_bk = [('BK', p, q, 0), ('BK', p, q, 1)]
            if pss == 0:
                for h in range(2):
                    rows = slice(64 * h, 64 * h + 64)
                    tp = (64 * h, 64 * h)
                    if own:
                        for g_ in range(2):
                            k.mm(psM[rows, g_, h, :], BKt[rows, 0, tc], ARt[rows, g_, tc], r=rk_ar + rk_bk, w=[('psprep', pb)], tp=tp)
                    else:
                        k.mm(psM[rows, 0, h, :], BKt[rows, 0, tc], ARt[rows, 0, tc], r=rk_ar + rk_bk, w=[('psprep', pb)], tp=tp)
                    k.mm(psM[rows, 2, h, :], ARt[rows, 0, tc], BKt[rows, 0, tc], r=rk_ar + rk_bk, w=[('psprep', pb)], tp=tp)
                    k.mm(psKT[rows, 0:64], BKt[rows, 0, tc], ident[rows, 64 * h:64 * h + 64], r=rk_bk + ['cb'], w=[('psprep', pb)], tp=tp)
                    k.mm(psKT[rows, 64:128], BKt[rows, 1, tc], ident[rows, 64 * h:64 * h + 64], r=rk_bk + ['cb'], w=[('psprep', pb)], tp=tp)
                k.tt('dve', PM[s][:], bank[:, 0:384], mask3, ALU.mult, r=[('psprep', pb), 'cb'], w=[('PM', s)])
                k.cp('dve', TK[s][:, 0:128], psKT, r=[('psprep', pb)], w=[('TK', s, 0)])
                k.tt('dve', TT1[s][:], PM[s][:, 0:128], ident, ALU.add, r=[('PM', s), 'cb'], w=[('TTf', s)])
            if pss == 1:
                psK = bank[:, 0:256].rearrange("p (g h t) -> p g h t", g=2, h=2)
                psV = bank[:, 384:448]
                for h in range(2):
                    rows = slice(64 * h, 64 * h + 64)
                    tp = (64 * h, 64 * h)
                    if own:
                        for g_ in range(2):
                            k.mm(psK[rows, g_, h, :], BKt[rows, 1, tc], ARt[rows, g_, tc], r=rk_ar + rk_bk, w=[('psprep', pb)], tp=tp)
                    else:
                        k.mm(psK[rows, 0, h, :], BKt[rows, 1, tc], ARt[rows, 0, tc], r=rk_ar + rk_bk, w=[('psprep', pb)], tp=tp)
                    k.mm(psV[rows, :], VBt[rows, tc], ident[rows, 64 * h:64 * h + 64], r=[('VB', p, q), 'cb'], w=[('psprep', pb)], tp=tp)
                k.tt('dve', MK[s][:], bank[:, 0:256], mask2, ALU.mult, r=[('psprep', pb), 'cb'], w=[('MK', s)])
                k.cp('dve', TK[s][:, 128:192], psV, r=[('psprep', pb)], w=[('TK', s, 1)])

    def dgroup(blk, c):
        sl0 = (c % 2) * 4
        cur = {}
        for p in range(4):
            s = sl0 + p
            cur[p] = dict(P=PM[s][:, 0:128], Pt=PM[s][:, 256:384], Tt=TT1[s][:], keys=[('PM', s), ('TTf', s)])
        for step in range(6):
            for p in range(4):
                s = sl0 + p
                u = cur[p]
                reg = PD[p][:, 0:384].rearrange("p (g c) -> p g c", g=3)
                pk = [PDK(p, 'd')]
                if False:
                    k.mm(PD[p][:, 0:256], u['Pt'], u['PT'], start=True, stop=False, r=u['keys'], w=pk)
                    k.mm(reg[:, 1, :], ident, u['Tt'], start=False, stop=True, r=u['keys'] + ['cb'], w=pk)
                    k.mm(reg[:, 2, :], u['P'], u['Pt'], r=u['keys'], w=pk)
                else:
                    if step > 0:
                        k.mm(reg[:, 1, :], ident, u['Tt'], start=True, stop=False, r=u['keys'] + ['cb'], w=pk)
                        k.mm(reg[:, 1, :], u['Pt'], u['Tt'], start=False, stop=True, r=u['keys'], w=pk)
                    if step < 5:
                        k.mm(reg[:, 0, :], u['Pt'], u['P'], r=u['keys'], w=pk)
                        k.mm(reg[:, 2, :], u['P'], u['Pt'], r=u['keys'], w=pk)
                eng = PENG[p]
                if step == 0:
                    d = DD[p][0]
                    dv = d[:, :].rearrange("p (g c) -> p g c", g=3)
                    k.cp(eng, dv[:, 0, :], reg[:, 0, :], r=pk, w=[('DD', p, 0, 'a')])
                    k.cp(eng, dv[:, 2, :], reg[:, 2, :], r=pk, w=[('DD', p, 0, 'b')])
                    cur[p] = dict(P=d[:, 0:128], Pt=d[:, 256:384], Tt=u['Tt'], keys=[('DD', p, 0, 'a'), ('DD', p, 0, 'b'), ('TTf', s)])
                elif step < 5:
                    d = DD[p][step % 2]
                    dv = d[:, :].rearrange("p (g c) -> p g c", g=3)
                    k.cp(eng, dv, reg, r=pk, w=[('DD', p, step % 2, 'a'), ('DD', p, step % 2, 'b')])
                    cur[p] = dict(P=d[:, 0:128], Pt=d[:, 256:384], Tt=d[:, 128:256], PT=d[:, 0:256], keys=[('DD', p, step % 2, 'a'), ('DD', p, step % 2, 'b')])
                else:
                    k.cp(eng, TTf[s][:], reg[:, 1, :], r=pk, w=[('TTf', s)])
            yield

    def chain(blk, c):
        own = blk >= OWN0
        q = blk % 2
        tc = slice(c * 64, (c + 1) * 64)
        sl0 = (c % 2) * 4
        for p in range(4):
            s = sl0 + p
            rX, rS = chain_regions(p)
            k.mm(rX, MK[s][:, 0:128], TK[s][:, 128:192], start=True, stop=False, r=[('MK', s), ('TK', s, 1)], w=[PDK(p, 'x')])
            for h in range(2):
                rows = slice(64 * h, 64 * h + 64)
                k.mm(rX[rows, :], AR[p][q][rows, 0, tc], Sbf[p][rows, :], start=False, stop=True, r=[('AR', p, q, 0), ('Sbf', p)], w=[PDK(p, 'x')], tp=(64 * h, 64 * h))
            k.cp(PENG[p], Xb[p][:], rX, r=[PDK(p, 'x')], w=[('Xb', p)])
        yield
        for p in range(4):
            s = sl0 + p
            rX, rS = chain_regions(p)
            k.mm(rX, TTf[s][:], Xb[p][:], r=[('TTf', s), ('Xb', p)], w=[PDK(p, 'x')])
            k.cp(PENG[p], Ub[p][:], rX, r=[PDK(p, 'x')], w=[('Ub', p)])
        yield
        for p in range(4):
            s = sl0 + p
            rX, rS = chain_regions(p)
            if own:
                for h in range(2):
                    rows = slice(64 * h, 64 * h + 64)
                    tp = (64 * h, 64 * h)
                    k.mm(rX[rows, :], Sbf[p][rows, :], AR[p][q][rows, 1, tc], start=True, stop=False, r=[('Sbf', p), ('AR', p, q, 1)], w=[PDK(p, 'x')], tp=tp)
                    k.mm(rX[rows, :], Ub[p][rows, :], PM[s][rows, 128 + 64 * h:128 + 64 * h + 64], start=False, stop=False, r=[('Ub', p), ('PM', s)], w=[PDK(p, 'x')], tp=tp)
                    k.mm(rX[rows, :], TK[s][rows, 128:192], MK[s][rows, 128 + 64 * h:128 + 64 * h + 64], start=False, stop=True, r=[('TK', s, 1), ('MK', s)], w=[PDK(p, 'x')], tp=tp)
                k.cp(PENG[p], Yb[p][:, tc], rX, r=[PDK(p, 'x')], w=[('Yb', p, c)])
            for h in range(2):
                rows = slice(64 * h, 64 * h + 64)
                tp = (64 * h, 64 * h)
                k.mm(rS[rows, :], TK[s][rows, 0:64], Ub[p][rows, :], start=True, stop=False, r=[('TK', s, 0), ('Ub', p)], w=[PDK(p, 's')], tp=tp)
                k.mm(rS[rows, :], TK[s][rows, 64:128], TK[s][rows, 128:192], start=False, stop=True, r=[('TK', s, 0), ('TK', s, 1)], w=[PDK(p, 's')], tp=tp)
            if PENG[p] == 'dve':
                k.tt('dve', Stmp[p][:], rS, S32[p][:], ALU.add, r=[PDK(p, 's'), ('S32', p)], w=[('Stmp', p)])
            else:
                k.cp('act', Stmp[p][:], rS, r=[PDK(p, 's')], w=[('Stmp', p)])
                k.tt('dve', Stmp[p][:], Stmp[p][:], S32[p][:], ALU.add, r=[('Stmp', p), ('S32', p)], w=[('Stmp', p)])
            wc = WC[p][q][:, c:c + 1]
            k.ts('dve', S32[p][:], Stmp[p][:], wc, ALU.mult, r=[('Stmp', p), ('WC', p, q)], w=[('S32', p)])
            k.act(Sbf[p][:], Stmp[p][:], AF.Copy, r=[('Stmp', p), ('WC', p, q)], w=[('Sbf', p)], scale=wc)
        yield

    def prep_all(blk, c):
        for p in range(4):
            prep_pass(blk, c, p, 0)
        for p in range(4):
            prep_pass(blk, c, p, 1)

    def scan_all(nblk):
        NG = nblk * 8
        bc = lambda g: (g // 8, g % 8)
        prep_all(*bc(0))
        if NG > 1:
            prep_all(*bc(1))
        for _ in dgroup(*bc(0)):
            pass
        fe = None
        for g in range(NG):
            blk, c = bc(g)
            if c == 0 and blk + 1 < nblk and DEV.get("KNOFE") is None:
                fe = frontend(blk + 1)
            d = dgroup(*bc(g + 1)) if g + 1 < NG else None
            ch = chain(blk, c)
            pq = []
            if g + 2 < NG:
                b2, c2 = bc(g + 2)
                pq = [(b2, c2, 0, 0), (b2, c2, 1, 0), (b2, c2, 0, 1), (b2, c2, 1, 1), (b2, c2, 2, 0), (b2, c2, 3, 0), (b2, c2, 2, 1), (b2, c2, 3, 1)]
            for step in range(6):
                if d is not None:
                    next(d, None)
                if step < 3:
                    next(ch, None)
                if step >= 2:
                    for _ in range(2):
                        if pq:
                            prep_pass(*pq.pop(0))
                if fe is not None:
                    for _ in range(4 if blk + 1 >= OWN0 else 3):
                        if next(fe, 'done') == 'done':
                            fe = None
                            break
            for _ in ch:
                pass
            if d is not None:
                for _ in d:
                    pass
            while pq:
                prep_pass(*pq.pop(0))
            if c == 5 and fe is not None:
                for _ in fe:
                    pass
                fe = None
            if c == 7 and blk >= OWN0:
                for _ in own_out(blk):
                    pass

    def own_out(blk):
        q = blk % 2
        t0 = (blk - OWN0) * BT
        T = tmp
        for p in range(4):
            YK = [('Yb', p, c) for c in range(8)]
            k.mm(PS[3][:, :], bones, Yb[p][:], r=['cb'] + YK, w=[('ps', 3)])
            k.stt(T["kk"][:], PS[3][:, :], -1.0 / 64, Yb[p][:], ALU.mult, ALU.add, r=[('ps', 3)] + YK, w=[('t', 0, 'kk')])
            k.act(sq[:], T["kk"][:], AF.Square, r=[('t', 0, 'kk')], w=[('sq', 0)])
            k.mm(PS[3][:, :], bones, sq[:], r=['cb', ('sq', 0)], w=[('ps', 3)])
            k.ts('dve', T["rn"][:], PS[3][:, :], 1.0 / 64, ALU.mult, 64e-5, ALU.add, r=[('ps', 3)], w=[('t', 0, 'rn')])
            k.act(T["rn"][:], T["rn"][:], AF.Sqrt, r=[('t', 0, 'rn')], w=[('t', 0, 'rn')])
            k.recip(T["rn"][:], T["rn"][:], r=[('t', 0, 'rn')], w=[('t', 0, 'rn')])
            k.tt('dve', T["kk"][:], T["kk"][:], T["rn"][:], ALU.mult, r=[('t', 0, 'kk'), ('t', 0, 'rn')], w=[('t', 0, 'kk')])
            k.ts('dve', T["kk"][:], T["kk"][:], pvc("ln_w", p), ALU.mult, pvc("ln_b", p), ALU.add, r=[('t', 0, 'kk'), 'pv'], w=[('t', 0, 'kk')])
            k.tt('dve', T["kk"][:], T["kk"][:], GB[p][q][:], ALU.mult, r=[('t', 0, 'kk'), ('GB', p, q)], w=[('t', 0, 'kk')])
            k.tt('dve', yrw[:, p, t0:t0 + BT], T["kk"][:], yrw[:, p, t0:t0 + BT], ALU.add, r=[('t', 0, 'kk'), ('yrw', p, blk - OWN0)], w=[('yrw', p, blk - OWN0)])
            yield

    pass
    STOP = DEV.get("KSTOP", "")
    def early():
        k.dma('pool', dbg_ap.rearrange("(p c) t -> c p t", p=4), yrw[:, :, :], r=[])
        fw.flush(final=True)
        A.close(); G.close(); fw.stack.close()
        return nc
    if STOP == "prep":
        return early()
    for _ in frontend(0):
        pass
    if STOP == "fe0":
        return early()
    if STOP == "scan0":
        scan_all(1)
        return early()
    if DEV.get("KFEONLY"):
        for b_ in range(1, NBLK):
            for _ in frontend(b_):
                pass
    else:
        scan_all(NBLK)
    if dbg == 'A':
        k.dma('pool', dbg_ap.rearrange("(p c) t -> c p t", p=4), yrw[:, :, :], r=[('yrw', p, b) for p in range(4) for b in range(4)])
        fw.flush(final=True)
        A.close(); G.close(); fw.stack.close()
        return nc
    fw.flush()
    A.close()

    BANKS = PS + PD
    bank_ctr = [0]

    def nb():
        i = bank_ctr[0] % 8
        bank_ctr[0] += 1
        return i, BANKS[i], ('bank', i)

    S_h2 = ExitStack()
    h2tm = k.sb(S_h2, "h2tm", [128, 16, 1024], BF16)
    ohg = k.sb(S_h2, "ohg", [128, 16, 8], F32)
    cwf = k.sb(S_h2, "cwf", [128, 16, 64], F32)
    KT = k.sb(S_h2, "KT", [128, 4, 256], BF16)
    Vm = k.sb(S_h2, "Vm", [128, 2, 512], BF16)
    cuh = k.sb(S_h2, "cuh", [128, 4, 2], F32)
    wrf = k.sb(S_h2, "wrf", [128, 8, 72], F32)
    idf = k.sb(S_h2, "idf", [128, 128], F32)
    bvr = k.sb(S_h2, "bvr", [128, 72], F32)
    bvg = k.sb(S_h2, "bvg", [128, 1024], F32)
    stat2 = k.sb(S_h2, "stat2", [128, 32], F32)
    k.dma('sp', wrf[:], wr.rearrange("(k p) n -> p k n", p=128), w=['wrf'])
    k.dma('sp', idf[:], consts[:, C_ID:C_ID + 128], w=['idf'])
    k.dma('sp', bvr[:], bvec[:, BV_RB:BV_RB + 72], w=['bvr'])
    k.dma('sp', bvg[:], bvec[:, BV_GN:BV_GN + 1024], w=['bvg'])

    def norm_tile(st, src_rows, xt_t, xn_t, key):
        k.dma('sp', xt_t[:], src_rows, w=[key + ('x',)])
        k.act(xn_t[:], xt_t[:], AF.Square, r=[key + ('x',)], w=[key + ('xn',), key + ('s0',)], accum_out=st[:, 0:1])
        k.ts('dve', st[:, 1:2], st[:, 0:1], 1.0 / 1024, ALU.mult, 1e-6, ALU.add, r=[key + ('s0',)], w=[key + ('s1',)])
        k.act(st[:, 2:3], st[:, 1:2], AF.Sqrt, r=[key + ('s1',)], w=[key + ('s2',)])
        k.recip(st[:, 3:4], st[:, 2:3], r=[key + ('s2',)], w=[key + ('s3',)])

    def norm_T(st, src_rows, xt_t, xn_t, key, dst, dst_key):
        norm_tile(st, src_rows, xt_t, xn_t, key)
        k.act(xn_t[:], xt_t[:], AF.Copy, r=[key + ('x',), key + ('s3',)], w=[key + ('xn',)], scale=st[:, 3:4])
        bi, bank, bkey = nb()
        psb = bank[:, :].bitcast(BF16)
        for kc in range(8):
            k.tr(psb[:, kc * 128:(kc + 1) * 128], xn_t[:, kc * 128:(kc + 1) * 128], ident, r=[key + ('xn',), 'cb'], w=[bkey])
        k.cp('act', dst, psb[:, 0:1024].rearrange("p (k t) -> p k t", k=8), w=[bkey, dst_key])

    with ExitStack() as B0:
        wkv = k.sb(B0, "wkv", [128, 8, 1024], BF16)
        memT = k.sb(B0, "memT", [128, 8, 256], BF16)
        xt0 = k.sb(B0, "xt0", [128, 1024], F32)
        xn0 = k.sb(B0, "xn0", [128, 1024], BF16)
        k.dma('pool', wkv[:], w_kv.rearrange("(k p) n -> p k n", p=128), w=['wkv'])
        for kc in range(8):
            k.ts('dve', wkv[:, kc, :], wkv[:, kc, :], pvc("g_mem", kc), ALU.mult, r=['pv', 'wkv'], w=['wkv'])
        for mt in range(2):
            norm_T(stat2, mem[mt * 128:(mt + 1) * 128, :], xt0, xn0, ('m0',), memT[:, :, mt * 128:(mt + 1) * 128], ('memT', mt))
        MKEYS = [('memT', 0), ('memT', 1)]
        for hd in range(4):
            bi, bank, bkey = nb()
            for kc in range(8):
                k.mm(bank[:, 0:256], wkv[:, kc, hd * 128:(hd + 1) * 128], memT[:, kc, :], start=(kc == 0), stop=(kc == 7), r=['wkv'] + MKEYS, w=[bkey])
            k.cp('act', KT[:, hd, :], bank[:, 0:256], w=[bkey, ('KT', hd)])
        for mt in range(2):
            bi, bank, bkey = nb()
            for kc in range(8):
                k.mm(bank[:, :], memT[:, kc, mt * 128:(mt + 1) * 128], wkv[:, kc, 512:1024], start=(kc == 0), stop=(kc == 7), r=['wkv'] + MKEYS, w=[bkey])
            k.cp('act', Vm[:, mt, :], bank[:, :], w=[bkey, ('Vm', mt)])
        fw.flush()
    KVK = [('KT', h) for h in range(4)] + [('Vm', m) for m in range(2)]

    HT = TOK // 2
    own_row0 = WIN - TOK

    def mixer_half(half):
        tok0 = half * HT
        S_z = ExitStack()
        zT = k.sb(S_z, "zT", [128, 8, HT], BF16)
        S_ab = ExitStack()
        hTa = k.sb(S_ab, "hTa", [128, 8, HT], BF16)
        yconv = k.sb(S_ab, "yconv", [128, 4, HT], BF16)
        ymem = k.sb(S_ab, "ymem", [128, 4, HT], BF16)
        with ExitStack() as Ba:
            wc = k.sb(Ba, "wc", [128, 8, 1536], BF16)
            wq = k.sb(Ba, "wq", [128, 8, 512], BF16)
            xtb = [k.sb(Ba, f"xtb{i}", [128, 1024], F32) for i in range(2)]
            xnb = [k.sb(Ba, f"xnb{i}", [128, 1024], BF16) for i in range(2)]
            stb = [k.sb(Ba, f"stb{i}", [128, 8], F32) for i in range(2)]
            hTh = k.sb(Ba, "hTh", [128, 8, 128], BF16)
            cgs2 = [k.sb(Ba, f"cgs{i}", [128, BT], F32) for i in range(2)]
            cue2 = [k.sb(Ba, f"cue{i}", [128, BT + 2], F32) for i in range(2)]
            cvt2 = [k.sb(Ba, f"cvt{i}", [128, BT], F32) for i in range(2)]
            qT2 = [k.sb(Ba, f"qT{i}", [128, BT], BF16) for i in range(2)]
            Eb2 = [k.sb(Ba, f"Eb{i}", [128, 2, BT], BF16) for i in range(2)]
            rden2 = [k.sb(Ba, f"rden{i}", [128, BT], F32) for i in range(2)]
            w_in_r = w_in.rearrange("(k p) n -> p k n", p=128)
            k.dma('pool', wc[:], w_in_r[:, :, 0:1536], w=['wc'])
            k.dma('pool', wq[:], w_in_r[:, :, 3072:3584], w=['wq'])
            for kc in range(8):
                k.ts('dve', wc[:, kc, :], wc[:, kc, :], pvc("g_mix", kc), ALU.mult, r=['pv', 'wc'], w=['wc'])
                k.ts('dve', wq[:, kc, :], wq[:, kc, :], pvc("g_mix", kc), ALU.mult, r=['pv', 'wq'], w=['wq'])

            def conv_chunk(c, hsrc, hkeys, ntok, t_out):
                cgs, cue, cvt = cgs2[c % 2], cue2[c % 2], cvt2[c % 2]
                CG, CU, CU0, CV = ('cgs', c % 2), ('cue', c % 2), ('cue0', c % 2), ('cvt', c % 2)
                bi, bank, bkey = nb()
                for kc in range(8):
                    k.mm(bank[:, 0:ntok], wc[:, kc, 512 + c * 128:512 + (c + 1) * 128], hsrc[:, kc, :], start=(kc == 0), stop=(kc == 7), r=['wc'] + hkeys, w=[bkey])
                k.cp('act', cgs[:, 0:ntok], bank[:, 0:ntok], w=[bkey, CG])
                bi, bank, bkey = nb()
                for kc in range(8):
                    k.mm(bank[:, 0:ntok], wc[:, kc, 1024 + c * 128:1024 + (c + 1) * 128], hsrc[:, kc, :], start=(kc == 0), stop=(kc == 7), r=['wc'] + hkeys, w=[bkey])
                k.tt('dve', cue[:, 2:2 + ntok], bank[:, 0:ntok], cgs[:, 0:ntok], ALU.mult, r=[CG], w=[bkey, CU])
                if t_out is not None:
                    k.cp('pool', cue[:, 0:2], cuh[:, c, :], r=[('cuh', c)], w=[CU0])
                    cwc = lambda j: pv[:, PV["conv_w"] + c * 3 + j:PV["conv_w"] + c * 3 + j + 1]
                    k.ts('dve', cvt[:, :], cue[:, 2:2 + BT], cwc(2), ALU.mult, r=[CU, 'pv'], w=[CV])
                    k.stt(cvt[:, :], cue[:, 1:1 + BT], cwc(1), cvt[:, :], ALU.mult, ALU.add, r=[CU, CU0, 'pv', CV], w=[CV])
                    k.stt(cvt[:, :], cue[:, 0:BT], cwc(0), cvt[:, :], ALU.mult, ALU.add, r=[CU, CU0, 'pv', CV], w=[CV])
                    bi, bank, bkey = nb()
                    for kc in range(8):
                        k.mm(bank[:, :], wc[:, kc, c * 128:(c + 1) * 128], hsrc[:, kc, :], start=(kc == 0), stop=(kc == 7), r=['wc'] + hkeys, w=[bkey])
                    k.tt('dve', yconv[:, c, t_out:t_out + BT], bank[:, :], cvt[:, :], ALU.mult, r=[CV], w=[bkey, ('yconv', c, t_out)])
                k.cp('pool', cuh[:, c, :], cue[:, ntok:ntok + 2], r=[CU, CU0], w=[('cuh', c)])

            if half == 0:
                r0 = own_row0 - 128
                norm_T(stb[0], xext[r0:r0 + 128, :], xtb[0], xnb[0], ('nt', 0), hTh[:, :, :], 'hTh')
                for c in range(4):
                    conv_chunk(c, hTh, ['hTh'], 128, None)
            for bi_ in range(HT // BT):
                t0 = bi_ * BT
                for tt_ in range(4):
                    r0 = own_row0 + tok0 + t0 + tt_ * 128
                    i2 = tt_ % 2
                    norm_T(stb[i2], xext[r0:r0 + 128, :], xtb[i2], xnb[i2], ('nt', i2), hTa[:, :, t0 + tt_ * 128:t0 + (tt_ + 1) * 128], ('hTa', bi_, tt_))
                hk = [('hTa', bi_, i) for i in range(4)]
                hv = hTa[:, :, t0:t0 + BT]
                for c in range(4):
                    conv_chunk(c, hv, hk, BT, t0)
                for hd in range(4):
                    qT, Eb, rden = qT2[hd % 2], Eb2[hd % 2], rden2[hd % 2]
                    QK, RDK = ('qT', hd % 2), ('rden', hd % 2)
                    EK = lambda m_: ('Eb', hd % 2, m_)
                    bi, bank, bkey = nb()
                    for kc in range(8):
                        k.mm(bank[:, :], wq[:, kc, hd * 128:(hd + 1) * 128], hv[:, kc, :], start=(kc == 0), stop=(kc == 7), r=['wq'] + hk, w=[bkey])
                    k.cp('act', qT[:, :], bank[:, :], w=[bkey, QK])
                    for mt in range(2):
                        bi, bank, bkey = nb()
                        k.mm(bank[:, :], KT[:, hd, mt * 128:(mt + 1) * 128], qT[:, :], r=KVK + [QK], w=[bkey])
                        k.act(Eb[:, mt, :], bank[:, :], AF.Exp, w=[bkey, EK(mt)], scale=float(128 ** -0.5))
                    bi, bank, bkey = nb()
                    for mt in range(2):
                        k.mm(bank[:, :], ones_b, Eb[:, mt, :], start=(mt == 0), stop=(mt == 1), r=['cb', EK(0), EK(1)], w=[bkey])
                    k.recip(rden[:, :], bank[:, :], w=[bkey, RDK])
                    bi, bank, bkey = nb()
                    for mt in range(2):
                        k.mm(bank[:, :], Vm[:, mt, hd * 128:(hd + 1) * 128], Eb[:, mt, :], start=(mt == 0), stop=(mt == 1), r=KVK + [EK(0), EK(1)], w=[bkey])
                    k.tt('dve', ymem[:, hd, t0:t0 + BT], bank[:, :], rden[:, :], ALU.mult, r=[RDK], w=[bkey, ('ymem', hd, t0)])
            fw.flush()
        with ExitStack() as Bb:
            Wg = [k.sb(Bb, f"Wg{i}", [128, 8, 1024], BF16) for i in range(2)]
            Wb = [k.sb(Bb, f"Wb{i}", [128, 4, 1024], BF16) for i in range(2)]
            gsb = [k.sb(Bb, f"gsb{i}", [128, BT], F32) for i in range(2)]
            ztmp = [k.sb(Bb, f"ztmp{i}", [128, BT], BF16) for i in range(2)]
            cnt = 0
            for i in range(3):
                wg_t, wb_t = Wg[i % 2], Wb[i % 2]
                k.dma('pool', wg_t[:], w_gate.rearrange("(k p) n -> p k n", p=128)[:, :, i * 1024:(i + 1) * 1024], w=[('Wg', i % 2)])
                k.dma('pool', wb_t[:], w_branch[i * 512:(i + 1) * 512, :].rearrange("(k p) n -> p k n", p=128), w=[('Wb', i % 2)])
                for kc in range(8):
                    k.ts('dve', wg_t[:, kc, :], wg_t[:, kc, :], pvc("g_mix", kc), ALU.mult, r=['pv', ('Wg', i % 2)], w=[('Wg', i % 2)])
                for bi_ in range(HT // BT):
                    t0 = bi_ * BT
                    if i == 0:
                        br = yconv[:, :, t0:t0 + BT]
                    elif i == 1:
                        br = yrw[:, :, tok0 + t0:tok0 + t0 + BT]
                    else:
                        br = ymem[:, :, t0:t0 + BT]
                    for n in range(8):
                        bi, bank, bkey = nb()
                        for kc in range(8):
                            k.mm(bank[:, :], wg_t[:, kc, n * 128:(n + 1) * 128], hTa[:, kc, t0:t0 + BT], start=(kc == 0), stop=(kc == 7), r=[('Wg', i % 2)], w=[bkey])
                        g_ = gsb[cnt % 2]
                        k.act(g_[:, :], bank[:, :], AF.Sigmoid, r=['pv'], w=[bkey, ('gsb', cnt % 2)], bias=pvc("b_gate", i * 8 + n))
                        bi, bank, bkey = nb()
                        for cc in range(4):
                            k.mm(bank[:, :], wb_t[:, cc, n * 128:(n + 1) * 128], br[:, cc, :], start=(cc == 0), stop=(cc == 3), r=[('Wb', i % 2)], w=[bkey])
                        if i == 0:
                            k.tt('dve', zT[:, n, t0:t0 + BT], bank[:, :], g_[:, :], ALU.mult, r=[('gsb', cnt % 2)], w=[bkey, ('zT', n, bi_)])
                        else:
                            zt = ztmp[cnt % 2]
                            k.tt('dve', zt[:, :], bank[:, :], g_[:, :], ALU.mult, r=[('gsb', cnt % 2)], w=[bkey, ('ztmp', cnt % 2)])
                            k.tt('pool', zT[:, n, t0:t0 + BT], zT[:, n, t0:t0 + BT], zt[:, :], ALU.add, r=[('ztmp', cnt % 2)], w=[('zT', n, bi_)])
                        cnt += 1
            fw.flush()
        S_ab.close()
        with ExitStack() as Bc:
            wo = k.sb(Bc, "wo", [128, 8, 1024], BF16)
            xtc = [k.sb(Bc, f"xtc{i}", [128, 1024], F32) for i in range(2)]
            x1t = [k.sb(Bc, f"x1t{i}", [128, 1024], F32) for i in range(2)]
            h2f = [k.sb(Bc, f"h2f{i}", [128, 1024], F32) for i in range(2)]
            junk2 = k.sb(Bc, "junk2", [128, 1024], BF16)
            h2Tf = k.sb(Bc, "h2Tf", [128, 8, 128], F32)
            lgs = k.sb(Bc, "lgs", [128, HT // 128, 72], F32)
            rs = k.sb(Bc, "rs", [128, HT // 128, 64], F32)
            k.dma('pool', wo[:], w_o.rearrange("(k p) n -> p k n", p=128), w=['wo'])
            for tt_ in range(HT // 128):
                i2 = tt_ % 2
                gt = half * (HT // 128) + tt_
                r0 = own_row0 + tok0 + tt_ * 128
                k.dma('sp', xtc[i2][:], xext[r0:r0 + 128, :], w=[('xtc', i2)])
                for nh in range(2):
                    bi, bank, bkey = nb()
                    for kc in range(8):
                        k.mm(bank[:, :], zT[:, kc, tt_ * 128:(tt_ + 1) * 128], wo[:, kc, nh * 512:(nh + 1) * 512], start=(kc == 0), stop=(kc == 7), r=['wo'], w=[bkey])
                    k.tt('dve', x1t[i2][:, nh * 512:(nh + 1) * 512], bank[:, :], xtc[i2][:, nh * 512:(nh + 1) * 512], ALU.add, r=[('xtc', i2)], w=[bkey, ('x1t', i2, nh)])
                X1K = [('x1t', i2, 0), ('x1t', i2, 1)]
                k.dma('sp', x1s[tok0 + tt_ * 128:tok0 + (tt_ + 1) * 128, :], x1t[i2][:], r=X1K, w=[('x1s', gt)])
                st = stat2
                k.act(junk2[:], x1t[i2][:], AF.Square, r=X1K, w=['junk2', 's0'], accum_out=st[:, 0:1])
                k.ts('dve', st[:, 1:2], st[:, 0:1], 1.0 / 1024, ALU.mult, 1e-6, ALU.add, r=['s0'], w=['s1'])
                k.act(st[:, 2:3], st[:, 1:2], AF.Sqrt, r=['s1'], w=['s2'])
                k.recip(st[:, 3:4], st[:, 2:3], r=['s2'], w=['s3'])
                h2 = h2f[i2]
                k.stt(h2[:], x1t[i2][:], st[:, 3:4], bvg[:], ALU.mult, ALU.mult, r=X1K + ['s3', 'bvg'], w=[('h2f', i2)])
                k.cp('act', h2tm[:, gt, :], h2[:], r=[('h2f', i2)], w=[('h2tm', gt)])
                for hf in range(2):
                    bi, bank, bkey = nb()
                    for kk_ in range(4):
                        kc = hf * 4 + kk_
                        k.tr(bank[:, kk_ * 128:(kk_ + 1) * 128], h2[:, kc * 128:(kc + 1) * 128], idf[:], r=[('h2f', i2), 'idf'], w=[bkey])
                    k.cp('act', h2Tf[:, hf * 4:(hf + 1) * 4, :], bank[:, :].rearrange("p (k t) -> p k t", k=4), w=[bkey, ('h2Tf', hf)])
                bi, bank, bkey = nb()
                for kc in range(8):
                    k.mm(bank[:, 0:72], h2Tf[:, kc, :], wrf[:, kc, :], start=(kc == 0), stop=(kc == 7), r=[('h2Tf', 0), ('h2Tf', 1), 'wrf'], w=[bkey])
                k.tt('dve', lgs[:, tt_, :], bank[:, 0:72], bvr[:, :], ALU.add, r=['bvr'], w=[bkey, ('lgs', tt_)])
                if dbg == 'B':
                    k.dma('sp', dbg2_ap[:, gt * 72:(gt + 1) * 72], lgs[:, tt_, :], r=[('lgs', tt_)])
                pass
            NTL = HT // 128
            def RR(t, i, n=1):
                return rs[:, t, i:i + n]
            def LG(t, a, b):
                return lgs[:, t, a:b]
            tiles = list(range(NTL))
            GT = lambda t: half * NTL + t
            RKt = lambda t: [('rs', t)]
            for t in tiles:
                fw.op('dve', lambda e, t=t: e.reduce_max(out=RR(t, 0), in_=LG(t, 0, 8), axis=AX.X), [('lgs', t)], RKt(t))
            for t in tiles:
                k.ts('dve', ohg[:, GT(t), :], LG(t, 0, 8), RR(t, 0), ALU.is_equal, r=[('lgs', t)] + RKt(t), w=[('ohg', GT(t))])
            for t in tiles:
                k.ts('dve', RR(t, 1), RR(t, 0), -1.0, ALU.mult, r=RKt(t), w=RKt(t))
            for t in tiles:
                k.act(RR(t, 8, 8), LG(t, 0, 8), AF.Exp, r=[('lgs', t)] + RKt(t), w=RKt(t), bias=RR(t, 1), accum_out=RR(t, 2))
            for t in tiles:
                k.recip(RR(t, 3), RR(t, 2), r=RKt(t), w=RKt(t))
            for t in tiles:
                k.ts('dve', RR(t, 16, 8), LG(t, 8, 16), ohg[:, GT(t), 0:1], ALU.mult, r=[('lgs', t), ('ohg', GT(t))] + RKt(t), w=RKt(t))
            for g_ in range(1, 8):
                for t in tiles:
                    k.stt(RR(t, 16, 8), LG(t, 8 + 8 * g_, 16 + 8 * g_), ohg[:, GT(t), g_:g_ + 1], RR(t, 16, 8), ALU.mult, ALU.add, r=[('lgs', t), ('ohg', GT(t))] + RKt(t), w=RKt(t))
            for t in tiles:
                fw.op('dve', lambda e, t=t: e.reduce_max(out=RR(t, 4), in_=RR(t, 16, 8), axis=AX.X), RKt(t), RKt(t))
            for t in tiles:
                k.ts('dve', RR(t, 5), RR(t, 4), -1.0, ALU.mult, r=RKt(t), w=RKt(t))
            for t in tiles:
                k.act(RR(t, 24, 8), RR(t, 16, 8), AF.Exp, r=RKt(t), w=RKt(t), bias=RR(t, 5))
            for t in tiles:
                fw.op('dve', lambda e, t=t: e.reduce_max(out=RR(t, 6), in_=RR(t, 24, 8), axis=AX.X), RKt(t), RKt(t))
            for t in tiles:
                k.ts('dve', RR(t, 32, 8), RR(t, 24, 8), RR(t, 6), ALU.is_equal, r=RKt(t), w=RKt(t))
            for t in tiles:
                k.stt(RR(t, 40, 8), RR(t, 32, 8), -4.0, RR(t, 24, 8), ALU.mult, ALU.add, r=RKt(t), w=RKt(t))
            for t in tiles:
                fw.op('dve', lambda e, t=t: e.reduce_max(out=RR(t, 7), in_=RR(t, 40, 8), axis=AX.X), RKt(t), RKt(t))
            for t in tiles:
                k.ts('dve', RR(t, 48, 8), RR(t, 40, 8), RR(t, 7), ALU.is_equal, r=RKt(t), w=RKt(t))
            for t in tiles:
                k.tt('dve', RR(t, 56), RR(t, 6), RR(t, 7), ALU.add, r=RKt(t), w=RKt(t))
            for t in tiles:
                k.recip(RR(t, 57), RR(t, 56), r=RKt(t), w=RKt(t))
            for t in tiles:
                k.tt('dve', RR(t, 57), RR(t, 57), RR(t, 3), ALU.mult, r=RKt(t), w=RKt(t))
            for t in tiles:
                k.tt('dve', RR(t, 58), RR(t, 6), RR(t, 57), ALU.mult, r=RKt(t), w=RKt(t))
            for t in tiles:
                k.tt('dve', RR(t, 59), RR(t, 7), RR(t, 57), ALU.mult, r=RKt(t), w=RKt(t))
            for t in tiles:
                k.ts('dve', RR(t, 32, 8), RR(t, 32, 8), RR(t, 58), ALU.mult, r=RKt(t), w=RKt(t))
            for t in tiles:
                k.stt(RR(t, 32, 8), RR(t, 48, 8), RR(t, 59), RR(t, 32, 8), ALU.mult, ALU.add, r=RKt(t), w=RKt(t))
            for g_ in range(8):
                for t in tiles:
                    k.ts('dve', cwf[:, GT(t), g_ * 8:(g_ + 1) * 8], RR(t, 32, 8), ohg[:, GT(t), g_:g_ + 1], ALU.mult, r=RKt(t) + [('ohg', GT(t))], w=[('cwf', GT(t))])
            fw.flush()
        S_z.close()

    mixer_half(0)
    mixer_half(1)
    if dbg == 'B':
        k.dma('sp', dbg_ap, x1s, r=[])
        fw.flush(final=True)
        S_h2.close(); G.close(); fw.stack.close()
        return nc

    with ExitStack() as C:
        iof = k.sb(C, "iof", [128, GCAP], F32)
        ohb = k.sb(C, "ohb", [128, 16, 8], BF16)
        chi = k.sb(C, "chi", [128, 16, 64], BF16)
        clo = k.sb(C, "clo", [128, 16, 64], BF16)
        pos = k.sb(C, "pos", [128, 16, 8], F32)
        SelG = k.sb(C, "SelG", [128, 16, GCAP], BF16)
        SelGT = k.sb(C, "SelGT", [128, 3, TOK], BF16)
        xgT = k.sb(C, "xgT", [128, 8, GCAP], BF16)
        cws = k.sb(C, "cws", [128, 3, 8], F32)
        yg = k.sb(C, "yg", [128, 3, 1024], F32)
        ygb = k.sb(C, "ygb", [128, 3, 1024], BF16)
        hid = k.sb(C, "hid", [128, 4, GCAP], BF16)
        sgt = [k.sb(C, f"sgt{i}", [128, GCAP], BF16) for i in range(2)]
        stg = [k.sb(C, f"stgc{i}", [128, 1024], F32) for i in range(2)]
        NWB = 3
        wgt = [k.sb(C, f"wgt{i}", [128, 8, 512], BF16) for i in range(NWB)]
        wut = [k.sb(C, f"wut{i}", [128, 8, 512], BF16) for i in range(NWB)]
        wdt = [k.sb(C, f"wdt{i}", [128, 4, 1024], BF16) for i in range(NWB)]
        k.dma('sp', iof[:], consts[:, C_IOTA:C_IOTA + GCAP], w=['iof'])
        OHK = [('ohg', t) for t in range(16)]
        CWK = [('cwf', t) for t in range(16)]
        k.cp('act', ohb[:], ohg[:], r=OHK, w=['ohb'])
        k.cp('act', chi[:], cwf[:], r=CWK, w=['chi'])
        k.tt('dve', clo[:], cwf[:], chi[:], ALU.subtract, r=CWK + ['chi'], w=['clo'])
        for t in range(16):
            bi, bank, bkey = nb()
            k.mm(bank[:, 0:8], lt_b, ohb[:, t, :], start=True, stop=(t == 0), r=['cb', 'ohb'], w=[bkey])
            for t2 in range(t):
                k.mm(bank[:, 0:8], ones_b, ohb[:, t2, :], start=False, stop=(t2 == t - 1), r=['cb', 'ohb'], w=[bkey])
            k.cp('act', pos[:, t, :], bank[:, 0:8], w=[bkey, ('pos', t)])
        wcnt = 0
        H2K = [('h2tm', t) for t in range(16)]

        def load_expert(E):
            i = E % NWB
            k.dma('pool', wgt[i][:], weg[E].rearrange("(k p) f -> p k f", p=128), w=[('wgt', i)])
            k.dma('pool', wut[i][:], weu[E].rearrange("(k p) f -> p k f", p=128), w=[('wut', i)])
            k.dma('pool', wdt[i][:], wed[E].rearrange("(k p) n -> p k n", p=128), w=[('wdt', i)])

        load_expert(0)
        load_expert(1)

        def build_sel(g):
            for t in range(16):
                k.ts('dve', SelG[:, t, :], iof[:, :], pos[:, t, g:g + 1], ALU.is_equal, ohg[:, t, g:g + 1], ALU.mult,
                     r=['iof', ('pos', t), ('ohg', t)], w=[('SelG', t)])

        build_sel(0)
        for g in range(8):
            SK = [('SelG', t) for t in range(16)]
            for kc in range(8):
                bi, bank, bkey = nb()
                for t in range(16):
                    k.mm(bank[:, 0:GCAP], h2tm[:, t, kc * 128:(kc + 1) * 128], SelG[:, t, :], start=(t == 0), stop=(t == 15), r=H2K + SK, w=[bkey])
                k.cp('act' if kc % 2 == 0 else 'dve', xgT[:, kc, :], bank[:, 0:GCAP], w=[bkey, ('xgT', kc)])
            XK = [('xgT', kc) for kc in range(8)]
            for st_ in range(3):
                bi, bank, bkey = nb()
                for t in range(16):
                    k.mm(bank[:, 0:8], SelG[:, t, st_ * 128:(st_ + 1) * 128], chi[:, t, g * 8:(g + 1) * 8], start=(t == 0), stop=False, r=SK + ['chi'], w=[bkey])
                    k.mm(bank[:, 0:8], SelG[:, t, st_ * 128:(st_ + 1) * 128], clo[:, t, g * 8:(g + 1) * 8], start=False, stop=(t == 15), r=SK + ['clo'], w=[bkey])
                k.cp('act', cws[:, st_, :], bank[:, 0:8], w=[bkey, ('cws', st_)])
            for st_ in range(3):
                for t8 in range(2):
                    bi, bank, bkey = nb()
                    psb = bank[:, :].bitcast(BF16)
                    for j in range(8):
                        t = t8 * 8 + j
                        k.tr(psb[:, j * 128:(j + 1) * 128], SelG[:, t, st_ * 128:(st_ + 1) * 128], ident, r=SK + ['cb'], w=[bkey])
                    k.cp('act', SelGT[:, st_, t8 * 1024:(t8 + 1) * 1024], psb[:, 0:1024], w=[bkey, ('SelGT', st_, t8)])
            if g + 1 < 8:
                build_sel(g + 1)
            for e_ in range(8):
                E = g * 8 + e_
                i = E % NWB
                if E + 2 < 64:
                    load_expert(E + 2)
                for fc in range(4):
                    bi, bank, bkey = nb()
                    for kc in range(8):
                        k.mm(bank[:, 0:GCAP], wgt[i][:, kc, fc * 128:(fc + 1) * 128], xgT[:, kc, :], start=(kc == 0), stop=(kc == 7), r=[('wgt', i)] + XK, w=[bkey])
                    sg = sgt[wcnt % 2]
                    k.act(sg[:, :], bank[:, 0:GCAP], AF.Silu, w=[bkey, ('sgt', wcnt % 2)])
                    bi, bank, bkey = nb()
                    for kc in range(8):
                        k.mm(bank[:, 0:GCAP], wut[i][:, kc, fc * 128:(fc + 1) * 128], xgT[:, kc, :], start=(kc == 0), stop=(kc == 7), r=[('wut', i)] + XK, w=[bkey])
                    k.tt('dve', hid[:, fc, :], bank[:, 0:GCAP], sg[:, :], ALU.mult, r=[('sgt', wcnt % 2)], w=[bkey, ('hid', fc)])
                    wcnt += 1
                HK_ = [('hid', fc) for fc in range(4)]
                for st_ in range(3):
                    for nh in range(2):
                        bi, bank, bkey = nb()
                        for fc in range(4):
                            k.mm(bank[:, :], hid[:, fc, st_ * 128:(st_ + 1) * 128], wdt[i][:, fc, nh * 512:(nh + 1) * 512], start=(fc == 0), stop=(fc == 3), r=HK_ + [('wdt', i)], w=[bkey])
                        ysl = yg[:, st_, nh * 512:(nh + 1) * 512]
                        if e_ == 0:
                            k.ts('dve', ysl, bank[:, :], cws[:, st_, e_:e_ + 1], ALU.mult, r=[('cws', st_)], w=[bkey, ('yg', st_, nh)])
                        else:
                            k.stt(ysl, bank[:, :], cws[:, st_, e_:e_ + 1], ysl, ALU.mult, ALU.add, r=[('cws', st_)], w=[bkey, ('yg', st_, nh)])
            YK_ = [('yg', a, b) for a in range(3) for b in range(2)]
            k.cp('act', ygb[:], yg[:], r=YK_, w=['ygb'])
            for t in range(16):
                sgi = t % 2
                for nh in range(2):
                    bi, bank, bkey = nb()
                    for st_ in range(3):
                        k.mm(bank[:, :], SelGT[:, st_, t * 128:(t + 1) * 128], ygb[:, st_, nh * 512:(nh + 1) * 512], start=(st_ == 0), stop=(st_ == 2),
                             r=['ygb'] + [('SelGT', a, b) for a in range(3) for b in range(2)], w=[bkey])
                    k.cp('act' if nh == 0 else 'dve', stg[sgi][:, nh * 512:(nh + 1) * 512], bank[:, :], w=[bkey, ('stg', sgi, nh)])
                fw.op('pool', lambda e, t=t, sgi=sgi: e.dma_start(out=x1s[t * 128:(t + 1) * 128, :], in_=stg[sgi][:], accum_op=ALU.add),
                      [('stg', sgi, 0), ('stg', sgi, 1)], [('x1s', t)], dma=True)
        fw.flush()

    with ExitStack() as D:
        gfb = k.sb(D, "gfb", [128, 1024], F32)
        xo = [k.sb(D, f"xo{i}", [128, 1024], F32) for i in range(2)]
        yo = [k.sb(D, f"yo{i}", [128, 1024], F32) for i in range(2)]
        jk = k.sb(D, "jk", [128, 1024], BF16)
        sd = [k.sb(D, f"sd{i}", [128, 8], F32) for i in range(2)]
        k.dma('sp', gfb[:], bvec[:, BV_GF:BV_GF + 1024], w=['gfb'])
        for t in range(16):
            i = t % 2
            st = sd[i]
            k.dma('sp', xo[i][:], x1s[t * 128:(t + 1) * 128, :], w=[('xo', i)])
            k.act(jk[:], xo[i][:], AF.Square, r=[('xo', i)], w=['jk', ('sd', i, 0)], accum_out=st[:, 0:1])
            k.ts('dve', st[:, 1:2], st[:, 0:1], 1.0 / 1024, ALU.mult, 1e-6, ALU.add, r=[('sd', i, 0)], w=[('sd', i, 1)])
            k.act(st[:, 2:3], st[:, 1:2], AF.Sqrt, r=[('sd', i, 1)], w=[('sd', i, 2)])
            k.recip(st[:, 3:4], st[:, 2:3], r=[('sd', i, 2)], w=[('sd', i, 3)])
            k.stt(yo[i][:], xo[i][:], st[:, 3:4], gfb[:], ALU.mult, ALU.mult, r=[('xo', i), ('sd', i, 3), 'gfb'], w=[('yo', i)])
            k.dma('sp', out[t * 128:(t + 1) * 128, :], yo[i][:], r=[('yo', i)], w=[('out', t)])
        fw.flush(final=True)
    S_h2.close()
    G.close()
    fw.stack.close()
    return nc


def _consts():
    c = np.zeros((128, NCON), np.float32)
    p = np.arange(128)[:, None]
    m = np.arange(128)[None, :]
    same = (p // 64) == (m // 64)
    c[:, C_ID:C_ID + 128] = np.eye(128)
    c[:, C_BO:C_BO + 128] = same
    c[:, C_M3:C_M3 + 128] = same & ((p % 64) < (m % 64))
    c[:, C_M3 + 128:C_M3 + 256] = same & ((p % 64) <= (m % 64))
    c[:, C_M3 + 256:C_M3 + 384] = same & ((p % 64) > (m % 64))
    c[:, C_RS:C_RS + 512] = (np.arange(512)[None, :] % 64 != 0)
    c[:, C_ONE:C_ONE + 128] = 1.0
    c[:, C_LT:C_LT + 128] = p < m
    c[:, C_IOTA:C_IOTA + GCAP] = np.arange(GCAP)[None, :]
    return c


def _fm(v):
    v = np.asarray(v, np.float32).reshape(-1, 128)
    return np.ascontiguousarray(v.T)


def prep_common(inp):
    f = lambda a: np.ascontiguousarray(np.asarray(a, np.float32))
    pv = np.zeros((128, NPV), np.float32)
    pv[:, PV["g_mix"]:PV["g_mix"] + 8] = _fm(inp["g_mix"][0])
    pv[:, PV["g_mem"]:PV["g_mem"] + 8] = _fm(inp["g_mem"][0])
    pv[:, PV["g_ffn"]:PV["g_ffn"] + 8] = _fm(inp["g_ffn"][0])
    for i, nm in enumerate(["mu_w", "mu_a", "mu_g"]):
        pv[:, PV[nm]:PV[nm] + 8] = _fm(inp["mu_wag"][0][i])
    pv[:, PV["b_gate"]:PV["b_gate"] + 24] = _fm(inp["b_gate"][0])
    cw = np.asarray(inp["conv_w"][0], np.float32).reshape(4, 128, 3)
    pv[:, PV["conv_w"]:PV["conv_w"] + 12] = cw.transpose(1, 0, 2).reshape(128, 12)
    for nm, key in [("k_k", "k_k"), ("k_a", "k_a"), ("w0", "w0"), ("a0", "a0"), ("ln_w", "ln_x_w"), ("ln_b", "ln_x_b")]:
        pv[:, PV[nm]:PV[nm] + 4] = _fm(inp[key][0])
    pv[:, PV["r_k"]:PV["r_k"] + 4] = _fm(np.asarray(inp["r_k"][0]).reshape(-1))
    bv = np.zeros((128, NBV), np.float32)
    bv[:, BV_MU:BV_MU + 1536] = np.asarray(inp["mu_rkv"][0], np.float32).reshape(1, 1536)
    bv[:, BV_GF:BV_GF + 1024] = np.asarray(inp["g_final"], np.float32).reshape(1, 1024)
    bv[:, BV_RB:BV_RB + 8] = np.asarray(inp["b_router_group"][0], np.float32).reshape(1, 8)
    bv[:, BV_RB + 8:BV_RB + 72] = np.asarray(inp["b_router_expert"][0], np.float32).reshape(1, 64)
    bv[:, BV_GN:BV_GN + 1024] = np.asarray(inp["g_ffn"][0], np.float32).reshape(1, 1024)
    com = dict(
        w_in=f(inp["w_in"][0]), w_gate=f(inp["w_gate"][0]), w_branch=f(np.asarray(inp["w_branch"][0]).reshape(1536, 1024)),
        w_o=f(inp["w_o"][0]), w_kv=f(inp["w_kv_mem"][0]),
        l1=f(np.concatenate([inp["w_lora1"][0], inp["a_lora1"][0], inp["g_lora1"][0]], axis=1)),
        l2wa=f(np.concatenate([inp["w_lora2"][0], inp["a_lora2"][0]], axis=0)),
        l2g=f(inp["g_lora2"][0]),
        wr=f(np.concatenate([inp["w_router_group"][0], inp["w_router_expert"][0]], axis=1)),
        weg=f(inp["w_exp_gate"][0]), weu=f(inp["w_exp_up"][0]), wed=f(inp["w_exp_down"][0]),
        pvec=pv, bvec=bv, consts=_consts(),
    )
    return com


def prep_core(inp, c, com):
    b, q = c // 4, c % 4
    x = np.asarray(inp["x"], np.float32)
    end = (q + 1) * TOK
    xe = np.zeros((WIN, 1024), np.float32)
    xe[WIN - end:] = x[b, :end]
    d = dict(com)
    d["xext"] = xe
    d["mem"] = np.ascontiguousarray(np.asarray(inp["mem"], np.float32)[b])
    return d


_NC_CACHE = {}


def kernel(**inputs):
    if "nc" not in _NC_CACHE:
        _NC_CACHE["nc"] = build_program()
    nc = _NC_CACHE["nc"]
    com = prep_common(inputs)
    in_maps = [prep_core(inputs, c, com) for c in range(NCORES)]
    res = run_bass_kernel_spmd(nc, in_maps, core_ids=list(range(NCORES)))
    out = np.zeros((2, 8192, 1024), np.float32)
    for c in range(NCORES):
        b, q = c // 4, c % 4
        out[b, q * TOK:(q + 1) * TOK] = res.results[c]["out"]
    return out
```

```python
import numpy as np
from contextlib import ExitStack
import concourse.bass as bass
import concourse.mybir as mybir
from concourse.bass_utils import run_bass_kernel_spmd

F32 = mybir.dt.float32
BF16 = mybir.dt.bfloat16
AF = mybir.ActivationFunctionType
ALU = mybir.AluOpType
AX = mybir.AxisListType

NCORES = 8
DEV = {}
TOK = 2048
WIN = 8192
BT = 512
NBLK = WIN // BT
OWN0 = NBLK - TOK // BT
CDEC = 0.6065306597126334
GCAP = 384

C_ID, C_BO, C_M3, C_RS, C_ONE, C_LT, C_IOTA = 0, 128, 256, 640, 1152, 1280, 1408
NCON = C_IOTA + GCAP
PV = {}
_o = 0
for _n, _w in [("g_mix", 8), ("g_mem", 8), ("g_ffn", 8), ("mu_w", 8), ("mu_a", 8), ("mu_g", 8), ("b_gate", 24),
               ("conv_w", 12), ("k_k", 4), ("k_a", 4), ("w0", 4), ("a0", 4), ("ln_w", 4), ("ln_b", 4), ("r_k", 4)]:
    PV[_n] = _o
    _o += _w
NPV = _o
BV_MU, BV_GF, BV_RB, BV_GN = 0, 1536, 2560, 2632
NBV = 2632 + 1024


class FW:
    EPOCH = 2000
    NDMA = 8

    def __init__(self, nc):
        self.nc = nc
        self.ops = []
        self.opmap = {}
        self.last_write = {}
        self.readers = {}
        self.engs = ['pe', 'act', 'dve', 'pool', 'sp']
        self.sems = {e: [] for e in self.engs}
        self.count = {e: 0 for e in self.engs}
        self.dma_sems = {}
        self.dma_n = {e: 0 for e in self.engs}
        self.dma_last = {}
        self.waited = {e: {} for e in self.engs}
        self.stack = ExitStack()
        self.barrier_tokens = []
        self.nid = 0
        self.n_inst = 0

    def _sem(self, name):
        return self.stack.enter_context(self.nc.semaphore(name))

    def op(self, eng, fn, reads=(), writes=(), dma=False):
        deps = set()
        for r in reads:
            if r in self.last_write:
                deps.add(self.last_write[r])
        for w in writes:
            if w in self.last_write:
                deps.add(self.last_write[w])
            for x in self.readers.get(w, ()):
                deps.add(x)
        oid = self.nid
        self.nid += 1
        o = dict(id=oid, eng=eng, fn=fn, deps=deps, dma=dma, sig=dma, token=None)
        if dma:
            slot = (eng, self.dma_n[eng] % self.NDMA)
            self.dma_n[eng] += 1
            if slot in self.dma_last:
                deps.add(self.dma_last[slot])
            self.dma_last[slot] = oid
            o['slot'] = slot
        if eng == 'pe' and not dma:
            o['deps'] = set(d for d in deps if not (d in self.opmap and self.opmap[d]['eng'] == 'pe' and not self.opmap[d]['dma']))
        self.ops.append(o)
        self.opmap[oid] = o
        for r in reads:
            self.readers.setdefault(r, []).append(oid)
        for w in writes:
            self.last_write[w] = oid
            self.readers[w] = []
        return oid

    def flush(self, final=False):
        nc = self.nc
        ops = self.ops
        ids = set(o['id'] for o in ops)
        for o in ops:
            o['deps'] = set(d for d in o['deps'] if d in ids)
            for d in o['deps']:
                self.opmap[d]['sig'] = True
        last = {}
        for o in ops:
            if not o['dma']:
                last[o['eng']] = o
        for o in last.values():
            o['sig'] = True
        for o in ops:
            if o['dma']:
                key = o['slot']
                if key not in self.dma_sems:
                    self.dma_sems[key] = [self._sem(f"dma_{key[0]}_{key[1]}"), 0]
                ent = self.dma_sems[key]
                ent[1] += 16
                o['token'] = (ent[0], ent[1], f"dma_{key[0]}_{key[1]}")
            elif o['sig']:
                e = o['eng']
                c = self.count[e]
                ep = c // self.EPOCH
                while len(self.sems[e]) <= ep:
                    self.sems[e].append(self._sem(f"s_{e}_{len(self.sems[e])}"))
                o['token'] = (self.sems[e][ep], c % self.EPOCH + 1, f"s_{e}_{ep}")
                self.count[e] = c + 1
        per_eng = {e: [o for o in ops if o['eng'] == e] for e in self.engs}
        start_tokens = list(self.barrier_tokens)
        end_tokens = [o['token'] for o in last.values()] + [o['token'] for o in ops if o['dma']]

        def emit(e, eng):
            waited = self.waited[e]

            def wait(tok):
                sem, val, name = tok
                if waited.get(name, 0) >= val:
                    return
                eng.wait_ge(sem, val)
                self.n_inst += 1
                waited[name] = val
            for t in start_tokens:
                wait(t)
            for o in per_eng[e]:
                for d in sorted(o['deps']):
                    wait(self.opmap[d]['token'])
                ins = o['fn'](eng)
                self.n_inst += 1
                if o['token'] is not None:
                    ins.then_inc(o['token'][0], 16 if o['dma'] else 1)
            if final and e == 'sp':
                for t in end_tokens:
                    wait(t)

        with nc.Block() as block:
            @block.tensor
            def _(eng):
                emit('pe', eng)

            @block.scalar
            def _(eng):
                emit('act', eng)

            @block.vector
            def _(eng):
                emit('dve', eng)

            @block.gpsimd
            def _(eng):
                emit('pool', eng)

            @block.sync
            def _(eng):
                emit('sp', eng)
        self.barrier_tokens = end_tokens
        for o in ops:
            o['fn'] = None
        self.ops = []
        self.opmap = {}
        self.last_write = {}
        self.readers = {}
        self.dma_last = {}


class K:
    def __init__(self, nc):
        self.nc = nc
        self.fw = FW(nc)
        self.uid = 0

    def sb(self, st, name, shape, dt):
        self.uid += 1
        return st.enter_context(self.nc.sbuf_tensor(f"{name}_{self.uid}", shape, dt))

    def ps(self, st, name, shape, dt=F32):
        self.uid += 1
        return st.enter_context(self.nc.psum_tensor(f"{name}_{self.uid}", shape, dt))

    def mm(self, out, lhsT, rhs, start=True, stop=True, r=(), w=(), tp=None):
        if tp is None:
            self.fw.op('pe', lambda e: e.matmul(out, lhsT=lhsT, rhs=rhs, start=start, stop=stop), r, w)
        else:
            self.fw.op('pe', lambda e: e.matmul(out, lhsT=lhsT, rhs=rhs, start=start, stop=stop, tile_position=tp), r, w)

    def tr(self, out, in_, ident, r=(), w=()):
        self.fw.op('pe', lambda e: e.transpose(out, in_, ident), r, w)

    def act(self, out, in_, func, r=(), w=(), **kw):
        self.fw.op('act', lambda e: e.activation(out=out, in_=in_, func=func, **kw), r, w)

    def tt(self, eng, out, in0, in1, op, r=(), w=()):
        self.fw.op(eng, lambda e: e.tensor_tensor(out=out, in0=in0, in1=in1, op=op), r, w)

    def ts(self, eng, out, in0, s1, op0, s2=None, op1=None, r=(), w=()):
        if op1 is None:
            self.fw.op(eng, lambda e: e.tensor_scalar(out=out, in0=in0, scalar1=s1, scalar2=None, op0=op0), r, w)
        else:
            self.fw.op(eng, lambda e: e.tensor_scalar(out=out, in0=in0, scalar1=s1, scalar2=s2, op0=op0, op1=op1), r, w)

    def stt(self, out, in0, scalar, in1, op0, op1, r=(), w=()):
        self.fw.op('dve', lambda e: e.scalar_tensor_tensor(out=out, in0=in0, scalar=scalar, in1=in1, op0=op0, op1=op1), r, w)

    def cp(self, eng, out, in_, r=(), w=()):
        if eng == 'act':
            self.act(out, in_, AF.Copy, r, w)
        else:
            self.fw.op(eng, lambda e: e.tensor_copy(out=out, in_=in_), r, w)

    def memset(self, eng, ap, val, w=()):
        self.fw.op(eng, lambda e: e.memset(ap, val), (), w)

    def recip(self, out, in_, r=(), w=()):
        self.fw.op('dve', lambda e: e.reciprocal(out=out, in_=in_), r, w)

    def dma(self, eng, out, in_, r=(), w=()):
        self.fw.op(eng, lambda e: e.dma_start(out=out, in_=in_), r, w, dma=True)


def run_interleaved(gens):
    gens = [g for g in gens if g is not None]
    while gens:
        for g in list(gens):
            try:
                next(g)
            except StopIteration:
                gens.remove(g)


def build_program(dbg=None):
    nc = bass.Bass("TRN2", target_bir_lowering=False)
    k = K(nc)
    fw = k.fw

    def din(name, shape):
        return nc.dram_tensor(name, shape, F32, kind="ExternalInput").ap()

    xext = din("xext", [WIN, 1024])
    mem = din("mem", [256, 1024])
    w_in = din("w_in", [1024, 3584])
    w_gate = din("w_gate", [1024, 3072])
    w_branch = din("w_branch", [1536, 1024])
    w_o = din("w_o", [1024, 1024])
    w_kv = din("w_kv", [1024, 1024])
    l1 = din("l1", [1024, 256])
    l2wa = din("l2wa", [128, 512])
    l2g = din("l2g", [128, 512])
    wr = din("wr", [1024, 72])
    if dbg is None or dbg == 'C':
        weg = din("weg", [64, 1024, 512])
        weu = din("weu", [64, 1024, 512])
        wed = din("wed", [64, 512, 1024])
    pvec = din("pvec", [128, NPV])
    bvec = din("bvec", [128, NBV])
    consts = din("consts", [128, NCON])
    out = nc.dram_tensor("out", [TOK, 1024], F32, kind="ExternalOutput").ap()
    x1s = nc.dram_tensor("x1s", [TOK, 1024], F32, kind="Internal").ap()
    dbg_ap = None
    if dbg == 'A':
        dbg_ap = nc.dram_tensor("dbg", [512, TOK], F32, kind="ExternalOutput").ap()
    if dbg == 'B':
        dbg_ap = nc.dram_tensor("dbg", [TOK, 1024], F32, kind="ExternalOutput").ap()
        dbg2_ap = nc.dram_tensor("dbg2", [128, 16 * 72], F32, kind="ExternalOutput").ap()

    G = ExitStack()
    cb = k.sb(G, "cb", [128, NCON], BF16)
    pv = k.sb(G, "pv", [128, NPV], F32)
    pvd = k.sb(G, "pvd", [128, 40], F32)
    yrw = k.sb(G, "yrw", [128, 4, TOK], BF16)
    k.dma('pool', cb[:], consts, w=['cb'])
    k.dma('sp', pv[:], pvec, w=['pv'])
    ident = cb[:, C_ID:C_ID + 128]
    bones = cb[:, C_BO:C_BO + 128]
    mask3 = cb[:, C_M3:C_M3 + 384]
    mask2 = cb[:, C_M3:C_M3 + 256]
    rsmask = cb[:, C_RS:C_RS + 512]
    ones_b = cb[:, C_ONE:C_ONE + 128]
    lt_b = cb[:, C_LT:C_LT + 128]
    iota_b = cb[:, C_IOTA:C_IOTA + GCAP]

    def pvc(name, i=0, n=1):
        return pv[:, PV[name] + i:PV[name] + i + n]

    for i, nm in enumerate(["mu_w", "mu_a", "mu_g"]):
        k.ts('dve', pvd[:, i * 8:(i + 1) * 8], pvc(nm, 0, 8), -1.0, ALU.mult, 1.0, ALU.add, r=['pv'], w=[('pvd', i)])
    k.ts('dve', pvd[:, 24:28], pvc("k_a", 0, 4), -1.0, ALU.mult, 1.0, ALU.add, r=['pv'], w=[('pvd', 3)])
    k.ts('dve', pvd[:, 28:32], pvc("a0", 0, 4), 0.5, ALU.mult, r=['pv'], w=[('pvd', 4)])
    k.ts('dve', pvd[:, 32:36], pvc("w0", 0, 4), 0.5, ALU.mult, r=['pv'], w=[('pvd', 5)])
    PVD_ALL = [('pvd', i) for i in range(6)]

    PS = [k.ps(G, f"bank{i}", [128, 512]) for i in range(4)]
    PD = [k.ps(G, f"pd{i}", [128, 512]) for i in range(4)]
    PENG = ['act', 'act', 'dve', 'dve']

    A = ExitStack()
    W1 = k.sb(A, "W1", [128, 8, 1536], BF16)
    W2 = k.sb(A, "W2", [128, 8, 1536], BF16)
    L1a = k.sb(A, "L1a", [128, 8, 256], BF16)
    L1b = k.sb(A, "L1b", [128, 8, 256], BF16)
    L2wa = k.sb(A, "L2wa", [128, 512], BF16)
    L2g = k.sb(A, "L2g", [128, 512], BF16)
    RKbd = k.sb(A, "RKbd", [128, 4, 128], BF16)
    k.dma('pool', L2wa[:], l2wa, w=['L2wa'])
    k.dma('pool', L2g[:], l2g, w=['L2g'])
    for p in range(4):
        k.ts('dve', RKbd[:, p, :], bones, pvc("r_k", p), ALU.mult, r=['cb', 'pv'], w=[('RKbd', p)])

    with ExitStack() as Wp:
        mub = k.sb(Wp, "mub", [128, 1536], F32)
        omub = k.sb(Wp, "omub", [128, 1536], F32)
        stg = [k.sb(Wp, f"stg{i}", [128, 1536], F32) for i in range(2)]
        l1s = k.sb(Wp, "l1s", [128, 8, 256], F32)
        k.dma('sp', mub[:], bvec[:, BV_MU:BV_MU + 1536], w=['mub'])
        k.ts('dve', omub[:], mub[:], -1.0, ALU.mult, 1.0, ALU.add, r=['mub'], w=['omub'])
        k.dma('sp', l1s[:], l1.rearrange("(k p) n -> p k n", p=128), w=['l1s'])
        for kc in range(8):
            s = stg[kc % 2]
            k.dma('sp', s[:], w_in[kc * 128:(kc + 1) * 128, 1536:3072], w=[('stg', kc % 2)])
            gk = pvc("g_mix", kc)
            k.stt(W1[:, kc, :], s[:], gk, omub[:], ALU.mult, ALU.mult, r=[('stg', kc % 2), 'omub', 'pv'], w=[('W1', kc)])
            k.stt(W2[:, kc, :], s[:], gk, mub[:], ALU.mult, ALU.mult, r=[('stg', kc % 2), 'mub', 'pv'], w=[('W2', kc)])
            for gi, (c0, c1) in enumerate([(0, 64), (64, 128), (128, 256)]):
                k.ts('dve', L1a[:, kc, c0:c1], l1s[:, kc, c0:c1], gk, ALU.mult, pvd[:, gi * 8 + kc:gi * 8 + kc + 1], ALU.mult,
                     r=['l1s', 'pv'] + PVD_ALL, w=[('L1a', kc, gi)])
                k.ts('dve', L1b[:, kc, c0:c1], l1s[:, kc, c0:c1], gk, ALU.mult, pv[:, PV["mu_w"] + gi * 8 + kc:PV["mu_w"] + gi * 8 + kc + 1], ALU.mult,
                     r=['l1s', 'pv'], w=[('L1b', kc, gi)])
        fw.flush()
    WKEYS = [('W1', kc) for kc in range(8)] + [('W2', kc) for kc in range(8)]
    LKEYS = [('L1a', kc, gi) for kc in range(8) for gi in range(3)] + [('L1b', kc, gi) for kc in range(8) for gi in range(3)]

    hT = [k.sb(A, "hT0", [128, 8, BT + 1], BF16)] * 2
    xt = [k.sb(A, "xt0", [128, 1024], BF16)] * 2
    xn = k.sb(A, "xn", [128, 1024], BF16)
    junk = xn
    stat = k.sb(A, "stat", [128, 8], F32)
    mhalf = k.sb(A, "mhalf", [128, 8], F32)
    k.memset('pool', mhalf[:], -0.5, w=['mhalf'])
    lwa = k.sb(A, "lwa", [128, BT], BF16)
    lg = k.sb(A, "lg", [128, BT], BF16)
    TMPN = ["kk", "rn", "a", "t", "kmod", "sig", "cum", "cp", "Wa", "Wb", "r32"]
    TBF = ("kk", "a", "t", "kmod", "Wa", "Wb", "r32")
    TS = [{n: k.sb(A, f"t{i}_" + n, [128, BT], BF16 if n in TBF else F32) for n in TMPN} for i in range(2)]
    SQ = [k.sb(A, "sq0", [128, BT], BF16)] * 2
    tmp = TS[0]
    sq = SQ[0]
    AR = [[k.sb(A, f"AR{p}{q}", [128, 2, BT], BF16) for q in range(2)] for p in range(4)]
    BK = [[k.sb(A, f"BK{p}{q}", [128, 2, BT], BF16) for q in range(2)] for p in range(4)]
    VB = [[k.sb(A, f"VB{p}{q}", [128, BT], BF16) for q in range(2)] for p in range(4)]
    WC = [[k.sb(A, f"WC{p}{q}", [128, 8], F32) for q in range(2)] for p in range(4)]
    GB = [[k.sb(A, f"GB{p}{q}", [128, BT], BF16) for q in range(2)] for p in range(4)]
    Yb = [k.sb(A, f"Yb{p}", [128, BT], BF16) for p in range(4)]
    NS = 8
    PM = [k.sb(A, f"PM{s}", [128, 384], BF16) for s in range(NS)]
    MK = [k.sb(A, f"MK{s}", [128, 256], BF16) for s in range(NS)]
    TK = [k.sb(A, f"TK{s}", [128, 192], BF16) for s in range(NS)]
    TTf = [k.sb(A, f"TTf{s}", [128, 128], BF16) for s in range(NS)]
    TT1 = TTf
    DD = [[k.sb(A, f"DD{s}{i}", [128, 384], BF16) for i in range(2)] for s in range(4)]
    S32 = [k.sb(A, f"S32{p}", [128, 64], F32) for p in range(4)]
    Sbf = [k.sb(A, f"Sbf{p}", [128, 64], BF16) for p in range(4)]
    Xb = [k.sb(A, f"Xb{p}", [128, 64], BF16) for p in range(4)]
    Ub = [k.sb(A, f"Ub{p}", [128, 64], BF16) for p in range(4)]
    Stmp = [k.sb(A, f"Stmp{p}", [128, 64], F32) for p in range(4)]
    for p in range(4):
        k.memset('pool', S32[p][:], 0.0, w=[('S32', p)])
        k.memset('pool', Sbf[p][:], 0.0, w=[('Sbf', p)])
    for b in (1, 2):
        k.memset('dve', PS[b][:, :], 0.0, w=[('psprep', b - 1)])

    def rmsnorm_T(src_rows, xt_i, dst, dst_key, gcols=None, f32dst=None):
        t = xt[xt_i]
        k.dma('pool', t[:], src_rows, w=[('xt', xt_i)])
        yield
        k.act(junk[:], t[:], AF.Square, r=[('xt', xt_i)], w=['xn', ('stat', 0)], accum_out=stat[:, 0:1])
        yield
        k.ts('dve', stat[:, 1:2], stat[:, 0:1], 1.0 / 1024, ALU.mult, 1e-6, ALU.add, r=[('stat', 0)], w=[('stat', 1)])
        yield
        k.tt('pool', stat[:, 3:4], stat[:, 1:2], mhalf[:, 0:1], ALU.pow, r=[('stat', 1), 'mhalf'], w=[('stat', 3)])
        yield
        k.act(xn[:], t[:], AF.Copy, r=[('xt', xt_i), ('stat', 3)], w=['xn'], scale=stat[:, 3:4])
        yield
        psb = PS[0][:, :].bitcast(BF16)
        for kc in range(8):
            k.tr(psb[:, kc * 128:(kc + 1) * 128], xn[:, kc * 128:(kc + 1) * 128], ident, r=['xn', 'cb'], w=[('ps', 0)])
        k.cp('act', dst, psb[:, 0:1024].rearrange("p (k t) -> p k t", k=8), r=[('ps', 0)], w=[dst_key])
        yield

    CH = [(p // 2, (p % 2) * 192) for p in range(4)]

    def PDK(p, what):
        return ('pd', p)

    def chain_regions(p):
        return PD[p][:, 384:448], PD[p][:, 448:512]

    def frontend(blk):
        own = blk >= OWN0
        q = blk % 2
        par = 0
        hb = hT[0]
        if blk == 0:
            k.memset('pool', hb[:, :, 0:1], 0.0, w=[('hT', par, 'c0')])
        else:
            k.cp('pool', hb[:, :, 0:1], hb[:, :, BT:BT + 1], r=[('hT', par, 3)], w=[('hT', par, 'c0')])
        for tt_ in range(4):
            row0 = blk * BT + tt_ * 128
            for _ in rmsnorm_T(xext[row0:row0 + 128, :], 0, hb[:, :, 1 + tt_ * 128:1 + (tt_ + 1) * 128], ('hT', par, tt_)):
                yield
        HK = [('hT', par, i) for i in range(4)] + [('hT', par, 'c0')]
        for m in range(2 if own else 1):
            for kc in range(8):
                k.mm(PS[0][:, :], L1a[:, kc, m * 128:(m + 1) * 128], hb[:, kc, 1:BT + 1], start=(kc == 0), stop=False, r=HK + LKEYS, w=[('ps', 0)])
            for kc in range(8):
                k.mm(PS[0][:, :], L1b[:, kc, m * 128:(m + 1) * 128], hb[:, kc, 0:BT], start=False, stop=(kc == 7), r=HK + LKEYS, w=[('ps', 0)])
            yield
            if m == 0:
                k.act(lwa[0:64, :], PS[0][0:64, :], AF.Tanh, r=[('ps', 0)], w=[('lwa', 0)])
                k.act(lwa[64:128, :], PS[0][64:128, :], AF.Copy, r=[('ps', 0)], w=[('lwa', 1)])
            else:
                k.act(lg[:], PS[0][:, :], AF.Sigmoid, r=[('ps', 0)], w=['lg'])
            yield

        def part1(p):
            si = p % 2
            T = TS[si]
            K_ = lambda n: ('t', si, n)
            pc = slice(p * 128, (p + 1) * 128)
            k.mm(PS[0][:, :], L2wa[64:128, pc], lwa[64:128, :], r=['L2wa', ('lwa', 1)], w=[('ps', 0)])
            k.act(T["a"][:], PS[0][:, :], AF.Tanh, r=[('ps', 0)] + PVD_ALL, w=[K_('a')], bias=pvd[:, 28 + p:29 + p], scale=0.5)
            yield
            k.ts('dve', T["a"][:], T["a"][:], 0.5, ALU.mult, 0.5, ALU.add, r=[K_('a')], w=[K_('a')])
            yield
            k.mm(PS[0][:, :], L2wa[0:64, pc], lwa[0:64, :], r=['L2wa', ('lwa', 0)], w=[('ps', 0)])
            k.act(T["sig"][:], PS[0][:, :], AF.Tanh, r=[('ps', 0)] + PVD_ALL, w=[K_('sig')], bias=pvd[:, 32 + p:33 + p], scale=0.5)
            yield
            k.ts('dve', T["sig"][:], T["sig"][:], 0.5, ALU.mult, 0.5, ALU.add, r=[K_('sig')], w=[K_('sig')])
            yield
            if own:
                k.mm(PS[0][:, :], L2g[:, pc], lg[:], r=['L2g', 'lg'], w=[('ps', 0)])
                k.act(GB[p][q][:], PS[0][:, :], AF.Copy, r=[('ps', 0)], w=[('GB', p, q)])
                yield
            yield
            k.ts('pool', T["t"][:], T["a"][:], pvc("k_a", p), ALU.mult, pvd[:, 24 + p:25 + p], ALU.add, r=[K_('a'), 'pv'] + PVD_ALL, w=[K_('t')])
            yield
            for qi, bank in ((1, 3), (2, 0), (0, 0)):
                if qi == 0 and not own:
                    continue
                for kc in range(8):
                    k.mm(PS[bank][:, :], W1[:, kc, qi * 512 + p * 128:qi * 512 + (p + 1) * 128], hb[:, kc, 1:BT + 1], start=(kc == 0), stop=False, r=HK + WKEYS, w=[('ps', bank)])
                for kc in range(8):
                    k.mm(PS[bank][:, :], W2[:, kc, qi * 512 + p * 128:qi * 512 + (p + 1) * 128], hb[:, kc, 0:BT], start=False, stop=(kc == 7), r=HK + WKEYS, w=[('ps', bank)])
                if qi == 1:
                    k.ts('dve', T["kk"][:], PS[bank][:, :], pvc("k_k", p), ALU.mult, r=[('ps', bank), 'pv'], w=[K_('kk')])
                    yield
                    k.tt('dve', T["kmod"][:], PS[bank][:, :], T["t"][:], ALU.mult, r=[('ps', bank), K_('t')], w=[K_('kmod')])
                    yield
                elif qi == 2:
                    k.cp('act', VB[p][q][:], PS[bank][:, :], r=[('ps', bank)], w=[('VB', p, q)])
                    yield
                else:
                    k.cp('act', T["r32"][:], PS[bank][:, :], r=[('ps', bank)], w=[K_('r32')])
                    yield
                yield

        def part2(p):
            si = p % 2
            T = TS[si]
            K_ = lambda n: ('t', si, n)
            sq_ = SQ[si]
            k.act(sq_[:], T["kk"][:], AF.Square, r=[K_('kk')], w=[('sq', 0)])
            yield
            k.mm(PS[3][:, :], bones, sq_[:], r=['cb', ('sq', 0)], w=[('ps', 3)])
            k.ts('dve', T["rn"][:], PS[3][:, :], 1e-24, ALU.max, r=[('ps', 3)], w=[K_('rn')])
            yield
            k.act(T["rn"][:], T["rn"][:], AF.Sqrt, r=[K_('rn')], w=[K_('rn')])
            yield
            k.recip(T["rn"][:], T["rn"][:], r=[K_('rn')], w=[K_('rn')])
            yield
            k.tt('pool', T["kk"][:], T["kk"][:], T["rn"][:], ALU.mult, r=[K_('kk'), K_('rn')], w=[K_('kk')])
            yield
            yield
            fw.op('dve', lambda e: e.tensor_tensor_scan(out=T["cum"][:], data0=rsmask, data1=T["sig"][:], initial=0.0, op0=ALU.mult, op1=ALU.add),
                  ['cb', K_('sig')], [K_('cum')])
            k.tt('pool', T["cp"][:], T["cum"][:], T["sig"][:], ALU.subtract, r=[K_('cum'), K_('sig')], w=[K_('cp')])
            yield
            cumv = T["cum"][:, :].rearrange("p (c t) -> p c t", t=64)
            k.act(WC[p][q][:, :], cumv[:, :, 63], AF.Exp, r=[K_('cum')], w=[('WC', p, q)], scale=-CDEC)
            yield
            k.act(T["Wa"][:], T["cp"][:], AF.Exp, r=[K_('cp')], w=[K_('Wa')], scale=-CDEC)
            yield
            k.stt(AR[p][q][:, 0, :], T["kk"][:], -1.0, T["Wa"][:], ALU.mult, ALU.mult, r=[K_('kk'), K_('Wa')], w=[('AR', p, q, 0)])
            yield
            yield
            k.act(T["Wb"][:], T["cum"][:], AF.Exp, r=[K_('cum')], w=[K_('Wb')], scale=CDEC)
            yield
            k.tt('pool', T["t"][:], T["kk"][:], T["a"][:], ALU.mult, r=[K_('kk'), K_('a'), K_('t')], w=[K_('t')])
            yield
            k.tt('dve', BK[p][q][:, 0, :], T["t"][:], T["Wb"][:], ALU.mult, r=[K_('t'), K_('Wb')], w=[('BK', p, q, 0)])
            yield
            k.tt('dve', BK[p][q][:, 1, :], T["kmod"][:], T["Wb"][:], ALU.mult, r=[K_('kmod'), K_('Wb')], w=[('BK', p, q, 1)])
            yield
            if own:
                k.act(T["Wa"][:], T["cum"][:], AF.Exp, r=[K_('cum'), K_('Wa')], w=[K_('Wa')], scale=-CDEC)
                yield
                k.tt('dve', AR[p][q][:, 1, :], T["r32"][:], T["Wa"][:], ALU.mult, r=[K_('r32'), K_('Wa')], w=[('AR', p, q, 1)])
                yield
                t0 = (blk - OWN0) * BT
                k.tt('pool', sq_[:], T["r32"][:], T["kmod"][:], ALU.mult, r=[K_('r32'), K_('kmod'), ('sq', 0)], w=[('sq', 0)])
                yield
                k.mm(PS[3][:, :], RKbd[:, p, :], sq_[:], r=[('RKbd', p), ('sq', 0)], w=[('ps', 3)])
                k.tt('dve', T["cp"][:], PS[3][:, :], VB[p][q][:], ALU.mult, r=[('ps', 3), ('VB', p, q)], w=[K_('cp')])
                yield
                k.tt('dve', yrw[:, p, t0:t0 + BT], T["cp"][:], GB[p][q][:], ALU.mult, r=[K_('cp'), ('GB', p, q)], w=[('yrw', p, blk - OWN0)])
                yield
            yield

        for _ in part1(0):
            yield
        for p_ in range(4):
            gens = [part2(p_)] + ([part1(p_ + 1)] if p_ + 1 < 4 else [])
            while gens:
                for g_ in list(gens):
                    if next(g_, 'done') == 'done':
                        gens.remove(g_)
                yield

    def prep_pass(blk, c, p, pss):
        own = blk >= OWN0
        q = blk % 2
        tc = slice(c * 64, (c + 1) * 64)
        sl0 = (c % 2) * 4
        if True:
            s = sl0 + p
            pb = p % 2
            bank = PS[1 + pb]
            psM = bank[:, 0:384].rearrange("p (g h t) -> p g h t", g=3, h=2)
            psKT = bank[:, 384:512]
            ARt, BKt, VBt = AR[p][q], BK[p][q], VB[p][q]
            rk_ar = [('AR', p, q, 0)] + ([('AR', p, q, 1)] if own else [])
            rk_bk = [('BK', p, q, 0), ('BK', p, q, 1)]
            if pss == 0:
                for h in range(2):
                    rows = slice(64 * h, 64 * h + 64)
                    tp = (64 * h, 64 * h)
                    if own:
                        for g_ in range(2):
                            k.mm(psM[rows, g_, h, :], BKt[rows, 0, tc], ARt[rows, g_, tc], r=rk_ar + rk_bk, w=[('psprep', pb)], tp=tp)
                    else:
                        k.mm(psM[rows, 0, h, :], BKt[rows, 0, tc], ARt[rows, 0, tc], r=rk_ar + rk_bk, w=[('psprep', pb)], tp=tp)
                    k.mm(psM[rows, 2, h, :], ARt[rows, 0, tc], BKt[rows, 0, tc], r=rk_ar + rk_bk, w=[('psprep', pb)], tp=tp)
                    k.mm(psKT[rows, 0:64], BKt[rows, 0, tc], ident[rows, 64 * h:64 * h + 64], r=rk_bk + ['cb'], w=[('psprep', pb)], tp=tp)
                    k.mm(psKT[rows, 64:128], BKt[rows, 1, tc], ident[rows, 64 * h:64 * h + 64], r=rk_bk + ['cb'], w=[('psprep', pb)], tp=tp)
                if own:
                    k.tt('dve', PM[s][:], bank[:, 0:384], mask3, ALU.mult, r=[('psprep', pb), 'cb'], w=[('PM', s)])
                else:
                    v3 = lambda ap: ap.rearrange("p (g c) -> p g c", g=3)[:, 0:3:2, :]
                    k.tt('dve', v3(PM[s][:, :]), v3(bank[:, 0:384]), v3(mask3), ALU.mult, r=[('psprep', pb), 'cb'], w=[('PM', s)])
                k.cp('dve', TK[s][:, 0:128], psKT, r=[('psprep', pb)], w=[('TK', s, 0)])
                k.tt('dve', TT1[s][:], PM[s][:, 0:128], ident, ALU.add, r=[('PM', s), 'cb'], w=[('TTf', s)])
            if pss == 1:
                psK = bank[:, 0:256].rearrange("p (g h t) -> p g h t", g=2, h=2)
                psV = bank[:, 384:448]
                for h in range(2):
                    rows = slice(64 * h, 64 * h + 64)
                    tp = (64 * h, 64 * h)
                    if own:
                        for g_ in range(2):
                            k.mm(psK[rows, g_, h, :], BKt[rows, 1, tc], ARt[rows, g_, tc], r=rk_ar + rk_bk, w=[('psprep', pb)], tp=tp)
                    else:
                        k.mm(psK[rows, 0, h, :], BKt[rows, 1, tc], ARt[rows, 0, tc], r=rk_ar + rk_bk, w=[('psprep', pb)], tp=tp)
                    k.mm(psV[rows, :], VBt[rows, tc], ident[rows, 64 * h:64 * h + 64], r=[('VB', p, q), 'cb'], w=[('psprep', pb)], tp=tp)
                if own:
                    k.tt('dve', MK[s][:], bank[:, 0:256], mask2, ALU.mult, r=[('psprep', pb), 'cb'], w=[('MK', s)])
                else:
                    k.tt('dve', MK[s][:, 0:128], bank[:, 0:128], mask2[:, 0:128], ALU.mult, r=[('psprep', pb), 'cb'], w=[('MK', s)])
                k.cp('dve', TK[s][:, 128:192], psV, r=[('psprep', pb)], w=[('TK', s, 1)])

    def dgroup(blk, c):
        sl0 = (c % 2) * 4
        cur = {}
        for p in range(4):
            s = sl0 + p
            cur[p] = dict(P=PM[s][:, 0:128], Pt=PM[s][:, 256:384], Tt=TT1[s][:], keys=[('PM', s), ('TTf', s)])
        for step in range(6):
            for p in range(4):
                s = sl0 + p
                u = cur[p]
                reg = PD[p][:, 0:384].rearrange("p (g c) -> p g c", g=3)
                pk = [PDK(p, 'd')]
                if False:
                    k.mm(PD[p][:, 0:256], u['Pt'], u['PT'], start=True, stop=False, r=u['keys'], w=pk)
                    k.mm(reg[:, 1, :], ident, u['Tt'], start=False, stop=True, r=u['keys'] + ['cb'], w=pk)
                    k.mm(reg[:, 2, :], u['P'], u['Pt'], r=u['keys'], w=pk)
                else:
                    if step > 0:
                        k.mm(reg[:, 1, :], ident, u['Tt'], start=True, stop=False, r=u['keys'] + ['cb'], w=pk)
                        k.mm(reg[:, 1, :], u['Pt'], u['Tt'], start=False, stop=True, r=u['keys'], w=pk)
                    if step < 5:
                        k.mm(reg[:, 0, :], u['Pt'], u['P'], r=u['keys'], w=pk)
                        k.mm(reg[:, 2, :], u['P'], u['Pt'], r=u['keys'], w=pk)
                eng = PENG[p]
                if step == 0:
                    d = DD[p][0]
                    dv = d[:, :].rearrange("p (g c) -> p g c", g=3)
                    k.cp(eng, dv[:, 0, :], reg[:, 0, :], r=pk, w=[('DD', p, 0, 'a')])
                    k.cp(eng, dv[:, 2, :], reg[:, 2, :], r=pk, w=[('DD', p, 0, 'b')])
                    cur[p] = dict(P=d[:, 0:128], Pt=d[:, 256:384], Tt=u['Tt'], keys=[('DD', p, 0, 'a'), ('DD', p, 0, 'b'), ('TTf', s)])
                elif step < 5:
                    d = DD[p][step % 2]
                    dv = d[:, :].rearrange("p (g c) -> p g c", g=3)
                    k.cp(eng, dv, reg, r=pk, w=[('DD', p, step % 2, 'a'), ('DD', p, step % 2, 'b')])
                    cur[p] = dict(P=d[:, 0:128], Pt=d[:, 256:384], Tt=d[:, 128:256], PT=d[:, 0:256], keys=[('DD', p, step % 2, 'a'), ('DD', p, step % 2, 'b')])
                else:
                    k.cp(eng, TTf[s][:], reg[:, 1, :], r=pk, w=[('TTf', s)])
            yield

    def chain(blk, c):
        own = blk >= OWN0
        q = blk % 2
        tc = slice(c * 64, (c + 1) * 64)
        sl0 = (c % 2) * 4
        for p in range(4):
            s = sl0 + p
            rX, rS = chain_regions(p)
            k.mm(rX, MK[s][:, 0:128], TK[s][:, 128:192], start=True, stop=False, r=[('MK', s), ('TK', s, 1)], w=[PDK(p, 'x')])
            for h in range(2):
                rows = slice(64 * h, 64 * h + 64)
                k.mm(rX[rows, :], AR[p][q][rows, 0, tc], Sbf[p][rows, :], start=False, stop=True, r=[('AR', p, q, 0), ('Sbf', p)], w=[PDK(p, 'x')], tp=(64 * h, 64 * h))
            k.cp(PENG[p], Xb[p][:], rX, r=[PDK(p, 'x')], w=[('Xb', p)])
        yield
        for p in range(4):
            s = sl0 + p
            rX, rS = chain_regions(p)
            k.mm(rX, TTf[s][:], Xb[p][:], r=[('TTf', s), ('Xb', p)], w=[PDK(p, 'x')])
            k.cp(PENG[p], Ub[p][:], rX, r=[PDK(p, 'x')], w=[('Ub', p)])
        yield
        for p in range(4):
            s = sl0 + p
            rX, rS = chain_regions(p)
            if own:
                for h in range(2):
                    rows = slice(64 * h, 64 * h + 64)
                    tp = (64 * h, 64 * h)
                    k.mm(rX[rows, :], Sbf[p][rows, :], AR[p][q][rows, 1, tc], start=True, stop=False, r=[('Sbf', p), ('AR', p, q, 1)], w=[PDK(p, 'x')], tp=tp)
                    k.mm(rX[rows, :], Ub[p][rows, :], PM[s][rows, 128 + 64 * h:128 + 64 * h + 64], start=False, stop=False, r=[('Ub', p), ('PM', s)], w=[PDK(p, 'x')], tp=tp)
                    k.mm(rX[rows, :], TK[s][rows, 128:192], MK[s][rows, 128 + 64 * h:128 + 64 * h + 64], start=False, stop=True, r=[('TK', s, 1), ('MK', s)], w=[PDK(p, 'x')], tp=tp)
                k.cp(PENG[p], Yb[p][:, tc], rX, r=[PDK(p, 'x')], w=[('Yb', p, c)])
            for h in range(2):
                rows = slice(64 * h, 64 * h + 64)
                tp = (64 * h, 64 * h)
                k.mm(rS[rows, :], TK[s][rows, 0:64], Ub[p][rows, :], start=True, stop=False, r=[('TK', s, 0), ('Ub', p)], w=[PDK(p, 's')], tp=tp)
                k.mm(rS[rows, :], TK[s][rows, 64:128], TK[s][rows, 128:192], start=False, stop=True, r=[('TK', s, 0), ('TK', s, 1)], w=[PDK(p, 's')], tp=tp)
            if PENG[p] == 'dve':
                k.tt('dve', Stmp[p][:], rS, S32[p][:], ALU.add, r=[PDK(p, 's'), ('S32', p)], w=[('Stmp', p)])
            else:
                k.cp('act', Stmp[p][:], rS, r=[PDK(p, 's')], w=[('Stmp', p)])
                k.tt('dve', Stmp[p][:], Stmp[p][:], S32[p][:], ALU.add, r=[('Stmp', p), ('S32', p)], w=[('Stmp', p)])
            wc = WC[p][q][:, c:c + 1]
            k.ts('dve', S32[p][:], Stmp[p][:], wc, ALU.mult, r=[('Stmp', p), ('WC', p, q)], w=[('S32', p)])
            k.act(Sbf[p][:], Stmp[p][:], AF.Copy, r=[('Stmp', p), ('WC', p, q)], w=[('Sbf', p)], scale=wc)
        yield

    def prep_all(blk, c):
        for p in range(4):
            prep_pass(blk, c, p, 0)
        for p in range(4):
            prep_pass(blk, c, p, 1)

    def scan_all(nblk):
        NG = nblk * 8
        bc = lambda g: (g // 8, g % 8)
        prep_all(*bc(0))
        if NG > 1:
            prep_all(*bc(1))
        for _ in dgroup(*bc(0)):
            pass
        fe = None
        for g in range(NG):
            blk, c = bc(g)
            if c == 0 and blk + 1 < nblk and DEV.get("KNOFE") is None:
                fe = frontend(blk + 1)
            d = dgroup(*bc(g + 1)) if g + 1 < NG else None
            ch = chain(blk, c)
            pq = []
            if g + 2 < NG:
                b2, c2 = bc(g + 2)
                pq = [(b2, c2, 0, 0), (b2, c2, 1, 0), (b2, c2, 0, 1), (b2, c2, 1, 1), (b2, c2, 2, 0), (b2, c2, 3, 0), (b2, c2, 2, 1), (b2, c2, 3, 1)]
            for step in range(6):
                if d is not None:
                    next(d, None)
                if step < 3:
                    next(ch, None)
                if step >= 2:
                    for _ in range(2):
                        if pq:
                            prep_pass(*pq.pop(0))
                if fe is not None:
                    for _ in range(4 if blk + 1 >= OWN0 else 3):
                        if next(fe, 'done') == 'done':
                            fe = None
                            break
            for _ in ch:
                pass
            if d is not None:
                for _ in d:
                    pass
            while pq:
                prep_pass(*pq.pop(0))
            if c == 5 and fe is not None:
                for _ in fe:
                    pass
                fe = None
            if c == 7 and blk >= OWN0:
                for _ in own_out(blk):
                    pass

    def own_out(blk):
        q = blk % 2
        t0 = (blk - OWN0) * BT
        T = tmp
        for p in range(4):
            YK = [('Yb', p, c) for c in range(8)]
            k.mm(PS[3][:, :], bones, Yb[p][:], r=['cb'] + YK, w=[('ps', 3)])
            k.stt(T["kk"][:], PS[3][:, :], -1.0 / 64, Yb[p][:], ALU.mult, ALU.add, r=[('ps', 3)] + YK, w=[('t', 0, 'kk')])
            k.act(sq[:], T["kk"][:], AF.Square, r=[('t', 0, 'kk')], w=[('sq', 0)])
            k.mm(PS[3][:, :], bones, sq[:], r=['cb', ('sq', 0)], w=[('ps', 3)])
            k.ts('dve', T["rn"][:], PS[3][:, :], 1.0 / 64, ALU.mult, 64e-5, ALU.add, r=[('ps', 3)], w=[('t', 0, 'rn')])
            k.act(T["rn"][:], T["rn"][:], AF.Sqrt, r=[('t', 0, 'rn')], w=[('t', 0, 'rn')])
            k.recip(T["rn"][:], T["rn"][:], r=[('t', 0, 'rn')], w=[('t', 0, 'rn')])
            k.tt('dve', T["kk"][:], T["kk"][:], T["rn"][:], ALU.mult, r=[('t', 0, 'kk'), ('t', 0, 'rn')], w=[('t', 0, 'kk')])
            k.ts('dve', T["kk"][:], T["kk"][:], pvc("ln_w", p), ALU.mult, pvc("ln_b", p), ALU.add, r=[('t', 0, 'kk'), 'pv'], w=[('t', 0, 'kk')])
            k.tt('dve', T["kk"][:], T["kk"][:], GB[p][q][:], ALU.mult, r=[('t', 0, 'kk'), ('GB', p, q)], w=[('t', 0, 'kk')])
            k.tt('dve', yrw[:, p, t0:t0 + BT], T["kk"][:], yrw[:, p, t0:t0 + BT], ALU.add, r=[('t', 0, 'kk'), ('yrw', p, blk - OWN0)], w=[('yrw', p, blk - OWN0)])
            yield

    pass
    STOP = DEV.get("KSTOP", "")
    def early():
        k.dma('pool', dbg_ap.rearrange("(p c) t -> c p t", p=4), yrw[:, :, :], r=[])
        fw.flush(final=True)
        A.close(); G.close(); fw.stack.close()
        return nc
    if STOP == "prep":
        return early()
    for _ in frontend(0):
        pass
    if STOP == "fe0":
        return early()
    if STOP == "scan0":
        scan_all(1)
        return early()
    if DEV.get("KFEONLY"):
        for b_ in range(1, NBLK):
            for _ in frontend(b_):
                pass
    else:
        scan_all(NBLK)
    if dbg == 'A':
        k.dma('pool', dbg_ap.rearrange("(p c) t -> c p t", p=4), yrw[:, :, :], r=[('yrw', p, b) for p in range(4) for b in range(4)])
        fw.flush(final=True)
        A.close(); G.close(); fw.stack.close()
        return nc
    fw.flush()
    A.close()

    BANKS = PS + PD
    bank_ctr = [0]

    def nb():
        i = bank_ctr[0] % 8
        bank_ctr[0] += 1
        return i, BANKS[i], ('bank', i)

    S_h2 = ExitStack()
    h2tm = k.sb(S_h2, "h2tm", [128, 16, 1024], BF16)
    ohg = k.sb(S_h2, "ohg", [128, 16, 8], F32)
    cwf = k.sb(S_h2, "cwf", [128, 16, 64], F32)
    KT = k.sb(S_h2, "KT", [128, 4, 256], BF16)
    Vm = k.sb(S_h2, "Vm", [128, 2, 512], BF16)
    cuh = k.sb(S_h2, "cuh", [128, 4, 2], F32)
    wrf = k.sb(S_h2, "wrf", [128, 8, 72], F32)
    idf = k.sb(S_h2, "idf", [128, 128], F32)
    bvr = k.sb(S_h2, "bvr", [128, 72], F32)
    bvg = k.sb(S_h2, "bvg", [128, 1024], F32)
    stat2 = k.sb(S_h2, "stat2", [128, 32], F32)
    k.dma('sp', wrf[:], wr.rearrange("(k p) n -> p k n", p=128), w=['wrf'])
    k.dma('sp', idf[:], consts[:, C_ID:C_ID + 128], w=['idf'])
    k.dma('sp', bvr[:], bvec[:, BV_RB:BV_RB + 72], w=['bvr'])
    k.dma('sp', bvg[:], bvec[:, BV_GN:BV_GN + 1024], w=['bvg'])

    def norm_tile(st, src_rows, xt_t, xn_t, key):
        k.dma('sp', xt_t[:], src_rows, w=[key + ('x',)])
        k.act(xn_t[:], xt_t[:], AF.Square, r=[key + ('x',)], w=[key + ('xn',), key + ('s0',)], accum_out=st[:, 0:1])
        k.ts('dve', st[:, 1:2], st[:, 0:1], 1.0 / 1024, ALU.mult, 1e-6, ALU.add, r=[key + ('s0',)], w=[key + ('s1',)])
        k.act(st[:, 2:3], st[:, 1:2], AF.Sqrt, r=[key + ('s1',)], w=[key + ('s2',)])
        k.recip(st[:, 3:4], st[:, 2:3], r=[key + ('s2',)], w=[key + ('s3',)])

    def norm_T(st, src_rows, xt_t, xn_t, key, dst, dst_key):
        norm_tile(st, src_rows, xt_t, xn_t, key)
        k.act(xn_t[:], xt_t[:], AF.Copy, r=[key + ('x',), key + ('s3',)], w=[key + ('xn',)], scale=st[:, 3:4])
        bi, bank, bkey = nb()
        psb = bank[:, :].bitcast(BF16)
        for kc in range(8):
            k.tr(psb[:, kc * 128:(kc + 1) * 128], xn_t[:, kc * 128:(kc + 1) * 128], ident, r=[key + ('xn',), 'cb'], w=[bkey])
        k.cp('act', dst, psb[:, 0:1024].rearrange("p (k t) -> p k t", k=8), w=[bkey, dst_key])

    with ExitStack() as B0:
        wkv = k.sb(B0, "wkv", [128, 8, 1024], BF16)
        memT = k.sb(B0, "memT", [128, 8, 256], BF16)
        xt0 = k.sb(B0, "xt0", [128, 1024], F32)
        xn0 = k.sb(B0, "xn0", [128, 1024], BF16)
        k.dma('pool', wkv[:], w_kv.rearrange("(k p) n -> p k n", p=128), w=['wkv'])
        for kc in range(8):
            k.ts('dve', wkv[:, kc, :], wkv[:, kc, :], pvc("g_mem", kc), ALU.mult, r=['pv', 'wkv'], w=['wkv'])
        for mt in range(2):
            norm_T(stat2, mem[mt * 128:(mt + 1) * 128, :], xt0, xn0, ('m0',), memT[:, :, mt * 128:(mt + 1) * 128], ('memT', mt))
        MKEYS = [('memT', 0), ('memT', 1)]
        for hd in range(4):
            bi, bank, bkey = nb()
            for kc in range(8):
                k.mm(bank[:, 0:256], wkv[:, kc, hd * 128:(hd + 1) * 128], memT[:, kc, :], start=(kc == 0), stop=(kc == 7), r=['wkv'] + MKEYS, w=[bkey])
            k.cp('act', KT[:, hd, :], bank[:, 0:256], w=[bkey, ('KT', hd)])
        for mt in range(2):
            bi, bank, bkey = nb()
            for kc in range(8):
                k.mm(bank[:, :], memT[:, kc, mt * 128:(mt + 1) * 128], wkv[:, kc, 512:1024], start=(kc == 0), stop=(kc == 7), r=['wkv'] + MKEYS, w=[bkey])
            k.cp('act', Vm[:, mt, :], bank[:, :], w=[bkey, ('Vm', mt)])
        fw.flush()
    KVK = [('KT', h) for h in range(4)] + [('Vm', m) for m in range(2)]

    HT = TOK // 2
    own_row0 = WIN - TOK

    def mixer_half(half):
        tok0 = half * HT
        S_z = ExitStack()
        zT = k.sb(S_z, "zT", [128, 8, HT], BF16)
        S_ab = ExitStack()
        hTa = k.sb(S_ab, "hTa", [128, 8, HT], BF16)
        yconv = k.sb(S_ab, "yconv", [128, 4, HT], BF16)
        ymem = k.sb(S_ab, "ymem", [128, 4, HT], BF16)
        with ExitStack() as Ba:
            wc = k.sb(Ba, "wc", [128, 8, 1536], BF16)
            wq = k.sb(Ba, "wq", [128, 8, 512], BF16)
            xtb = [k.sb(Ba, f"xtb{i}", [128, 1024], F32) for i in range(2)]
            xnb = [k.sb(Ba, f"xnb{i}", [128, 1024], BF16) for i in range(2)]
            stb = [k.sb(Ba, f"stb{i}", [128, 8], F32) for i in range(2)]
            hTh = k.sb(Ba, "hTh", [128, 8, 128], BF16)
            cgs2 = [k.sb(Ba, f"cgs{i}", [128, BT], F32) for i in range(2)]
            cue2 = [k.sb(Ba, f"cue{i}", [128, BT + 2], F32) for i in range(2)]
            cvt2 = [k.sb(Ba, f"cvt{i}", [128, BT], F32) for i in range(2)]
            qT2 = [k.sb(Ba, f"qT{i}", [128, BT], BF16) for i in range(2)]
            Eb2 = [k.sb(Ba, f"Eb{i}", [128, 2, BT], BF16) for i in range(2)]
            rden2 = [k.sb(Ba, f"rden{i}", [128, BT], F32) for i in range(2)]
            w_in_r = w_in.rearrange("(k p) n -> p k n", p=128)
            k.dma('pool', wc[:], w_in_r[:, :, 0:1536], w=['wc'])
            k.dma('pool', wq[:], w_in_r[:, :, 3072:3584], w=['wq'])
            for kc in range(8):
                k.ts('dve', wc[:, kc, :], wc[:, kc, :], pvc("g_mix", kc), ALU.mult, r=['pv', 'wc'], w=['wc'])
                k.ts('dve', wq[:, kc, :], wq[:, kc, :], pvc("g_mix", kc), ALU.mult, r=['pv', 'wq'], w=['wq'])

            def conv_chunk(c, hsrc, hkeys, ntok, t_out):
                cgs, cue, cvt = cgs2[c % 2], cue2[c % 2], cvt2[c % 2]
                CG, CU, CU0, CV = ('cgs', c % 2), ('cue', c % 2), ('cue0', c % 2), ('cvt', c % 2)
                bi, bank, bkey = nb()
                for kc in range(8):
                    k.mm(bank[:, 0:ntok], wc[:, kc, 512 + c * 128:512 + (c + 1) * 128], hsrc[:, kc, :], start=(kc == 0), stop=(kc == 7), r=['wc'] + hkeys, w=[bkey])
                k.cp('act', cgs[:, 0:ntok], bank[:, 0:ntok], w=[bkey, CG])
                bi, bank, bkey = nb()
                for kc in range(8):
                    k.mm(bank[:, 0:ntok], wc[:, kc, 1024 + c * 128:1024 + (c + 1) * 128], hsrc[:, kc, :], start=(kc == 0), stop=(kc == 7), r=['wc'] + hkeys, w=[bkey])
                k.tt('dve', cue[:, 2:2 + ntok], bank[:, 0:ntok], cgs[:, 0:ntok], ALU.mult, r=[CG], w=[bkey, CU])
                if t_out is not None:
                    k.cp('pool', cue[:, 0:2], cuh[:, c, :], r=[('cuh', c)], w=[CU0])
                    cwc = lambda j: pv[:, PV["conv_w"] + c * 3 + j:PV["conv_w"] + c * 3 + j + 1]
                    k.ts('dve', cvt[:, :], cue[:, 2:2 + BT], cwc(2), ALU.mult, r=[CU, 'pv'], w=[CV])
                    k.stt(cvt[:, :], cue[:, 1:1 + BT], cwc(1), cvt[:, :], ALU.mult, ALU.add, r=[CU, CU0, 'pv', CV], w=[CV])
                    k.stt(cvt[:, :], cue[:, 0:BT], cwc(0), cvt[:, :], ALU.mult, ALU.add, r=[CU, CU0, 'pv', CV], w=[CV])
                    bi, bank, bkey = nb()
                    for kc in range(8):
                        k.mm(bank[:, :], wc[:, kc, c * 128:(c + 1) * 128], hsrc[:, kc, :], start=(kc == 0), stop=(kc == 7), r=['wc'] + hkeys, w=[bkey])
                    k.tt('dve', yconv[:, c, t_out:t_out + BT], bank[:, :], cvt[:, :], ALU.mult, r=[CV], w=[bkey, ('yconv', c, t_out)])
                k.cp('pool', cuh[:, c, :], cue[:, ntok:ntok + 2], r=[CU, CU0], w=[('cuh', c)])

            if half == 0:
                r0 = own_row0 - 128
                norm_T(stb[0], xext[r0:r0 + 128, :], xtb[0], xnb[0], ('nt', 0), hTh[:, :, :], 'hTh')
                for c in range(4):
                    conv_chunk(c, hTh, ['hTh'], 128, None)
            for bi_ in range(HT // BT):
                t0 = bi_ * BT
                for tt_ in range(4):
                    r0 = own_row0 + tok0 + t0 + tt_ * 128
                    i2 = tt_ % 2
                    norm_T(stb[i2], xext[r0:r0 + 128, :], xtb[i2], xnb[i2], ('nt', i2), hTa[:, :, t0 + tt_ * 128:t0 + (tt_ + 1) * 128], ('hTa', bi_, tt_))
                hk = [('hTa', bi_, i) for i in range(4)]
                hv = hTa[:, :, t0:t0 + BT]
                for c in range(4):
                    conv_chunk(c, hv, hk, BT, t0)
                for hd in range(4):
                    qT, Eb, rden = qT2[hd % 2], Eb2[hd % 2], rden2[hd % 2]
                    QK, RDK = ('qT', hd % 2), ('rden', hd % 2)
                    EK = lambda m_: ('Eb', hd % 2, m_)
                    bi, bank, bkey = nb()
                    for kc in range(8):
                        k.mm(bank[:, :], wq[:, kc, hd * 128:(hd + 1) * 128], hv[:, kc, :], start=(kc == 0), stop=(kc == 7), r=['wq'] + hk, w=[bkey])
                    k.cp('act', qT[:, :], bank[:, :], w=[bkey, QK])
                    for mt in range(2):
                        bi, bank, bkey = nb()
                        k.mm(bank[:, :], KT[:, hd, mt * 128:(mt + 1) * 128], qT[:, :], r=KVK + [QK], w=[bkey])
                        k.act(Eb[:, mt, :], bank[:, :], AF.Exp, w=[bkey, EK(mt)], scale=float(128 ** -0.5))
                    bi, bank, bkey = nb()
                    for mt in range(2):
                        k.mm(bank[:, :], ones_b, Eb[:, mt, :], start=(mt == 0), stop=(mt == 1), r=['cb', EK(0), EK(1)], w=[bkey])
                    k.recip(rden[:, :], bank[:, :], w=[bkey, RDK])
                    bi, bank, bkey = nb()
                    for mt in range(2):
                        k.mm(bank[:, :], Vm[:, mt, hd * 128:(hd + 1) * 128], Eb[:, mt, :], start=(mt == 0), stop=(mt == 1), r=KVK + [EK(0), EK(1)], w=[bkey])
                    k.tt('dve', ymem[:, hd, t0:t0 + BT], bank[:, :], rden[:, :], ALU.mult, r=[RDK], w=[bkey, ('ymem', hd, t0)])
            fw.flush()
        with ExitStack() as Bb:
            Wg = [k.sb(Bb, f"Wg{i}", [128, 8, 1024], BF16) for i in range(2)]
            Wb = [k.sb(Bb, f"Wb{i}", [128, 4, 1024], BF16) for i in range(2)]
            gsb = [k.sb(Bb, f"gsb{i}", [128, BT], F32) for i in range(2)]
            ztmp = [k.sb(Bb, f"ztmp{i}", [128, BT], BF16) for i in range(2)]
            cnt = 0
            for i in range(3):
                wg_t, wb_t = Wg[i % 2], Wb[i % 2]
                k.dma('pool', wg_t[:], w_gate.rearrange("(k p) n -> p k n", p=128)[:, :, i * 1024:(i + 1) * 1024], w=[('Wg', i % 2)])
                k.dma('pool', wb_t[:], w_branch[i * 512:(i + 1) * 512, :].rearrange("(k p) n -> p k n", p=128), w=[('Wb', i % 2)])
                for kc in range(8):
                    k.ts('dve', wg_t[:, kc, :], wg_t[:, kc, :], pvc("g_mix", kc), ALU.mult, r=['pv', ('Wg', i % 2)], w=[('Wg', i % 2)])
                for bi_ in range(HT // BT):
                    t0 = bi_ * BT
                    if i == 0:
                        br = yconv[:, :, t0:t0 + BT]
                    elif i == 1:
                        br = yrw[:, :, tok0 + t0:tok0 + t0 + BT]
                    else:
                        br = ymem[:, :, t0:t0 + BT]
                    for n in range(8):
                        bi, bank, bkey = nb()
                        for kc in range(8):
                            k.mm(bank[:, :], wg_t[:, kc, n * 128:(n + 1) * 128], hTa[:, kc, t0:t0 + BT], start=(kc == 0), stop=(kc == 7), r=[('Wg', i % 2)], w=[bkey])
                        g_ = gsb[cnt % 2]
                        k.act(g_[:, :], bank[:, :], AF.Sigmoid, r=['pv'], w=[bkey, ('gsb', cnt % 2)], bias=pvc("b_gate", i * 8 + n))
                        bi, bank, bkey = nb()
                        for cc in range(4):
                            k.mm(bank[:, :], wb_t[:, cc, n * 128:(n + 1) * 128], br[:, cc, :], start=(cc == 0), stop=(cc == 3), r=[('Wb', i % 2)], w=[bkey])
                        if i == 0:
                            k.tt('dve', zT[:, n, t0:t0 + BT], bank[:, :], g_[:, :], ALU.mult, r=[('gsb', cnt % 2)], w=[bkey, ('zT', n, bi_)])
                        else:
                            zt = ztmp[cnt % 2]
                            k.tt('dve', zt[:, :], bank[:, :], g_[:, :], ALU.mult, r=[('gsb', cnt % 2)], w=[bkey, ('ztmp', cnt % 2)])
                            k.tt('pool', zT[:, n, t0:t0 + BT], zT[:, n, t0:t0 + BT], zt[:, :], ALU.add, r=[('ztmp', cnt % 2)], w=[('zT', n, bi_)])
                        cnt += 1
            fw.flush()
        S_ab.close()
        with ExitStack() as Bc:
            wo = k.sb(Bc, "wo", [128, 8, 1024], BF16)
            xtc = [k.sb(Bc, f"xtc{i}", [128, 1024], F32) for i in range(2)]
            x1t = [k.sb(Bc, f"x1t{i}", [128, 1024], F32) for i in range(2)]
            h2f = [k.sb(Bc, f"h2f{i}", [128, 1024], F32) for i in range(2)]
            junk2 = k.sb(Bc, "junk2", [128, 1024], BF16)
            h2Tf = k.sb(Bc, "h2Tf", [128, 8, 128], F32)
            lgs = k.sb(Bc, "lgs", [128, HT // 128, 72], F32)
            rs = k.sb(Bc, "rs", [128, HT // 128, 64], F32)
            k.dma('pool', wo[:], w_o.rearrange("(k p) n -> p k n", p=128), w=['wo'])
            for tt_ in range(HT // 128):
                i2 = tt_ % 2
                gt = half * (HT // 128) + tt_
                r0 = own_row0 + tok0 + tt_ * 128
                k.dma('sp', xtc[i2][:], xext[r0:r0 + 128, :], w=[('xtc', i2)])
                for nh in range(2):
                    bi, bank, bkey = nb()
                    for kc in range(8):
                        k.mm(bank[:, :], zT[:, kc, tt_ * 128:(tt_ + 1) * 128], wo[:, kc, nh * 512:(nh + 1) * 512], start=(kc == 0), stop=(kc == 7), r=['wo'], w=[bkey])
                    k.tt('dve', x1t[i2][:, nh * 512:(nh + 1) * 512], bank[:, :], xtc[i2][:, nh * 512:(nh + 1) * 512], ALU.add, r=[('xtc', i2)], w=[bkey, ('x1t', i2, nh)])
                X1K = [('x1t', i2, 0), ('x1t', i2, 1)]
                k.dma('sp', x1s[tok0 + tt_ * 128:tok0 + (tt_ + 1) * 128, :], x1t[i2][:], r=X1K, w=[('x1s', gt)])
                st = stat2
                k.act(junk2[:], x1t[i2][:], AF.Square, r=X1K, w=['junk2', 's0'], accum_out=st[:, 0:1])
                k.ts('dve', st[:, 1:2], st[:, 0:1], 1.0 / 1024, ALU.mult, 1e-6, ALU.add, r=['s0'], w=['s1'])
                k.act(st[:, 2:3], st[:, 1:2], AF.Sqrt, r=['s1'], w=['s2'])
                k.recip(st[:, 3:4], st[:, 2:3], r=['s2'], w=['s3'])
                h2 = h2f[i2]
                k.stt(h2[:], x1t[i2][:], st[:, 3:4], bvg[:], ALU.mult, ALU.mult, r=X1K + ['s3', 'bvg'], w=[('h2f', i2)])
                k.cp('act', h2tm[:, gt, :], h2[:], r=[('h2f', i2)], w=[('h2tm', gt)])
                for hf in range(2):
                    bi, bank, bkey = nb()
                    for kk_ in range(4):
                        kc = hf * 4 + kk_
                        k.tr(bank[:, kk_ * 128:(kk_ + 1) * 128], h2[:, kc * 128:(kc + 1) * 128], idf[:], r=[('h2f', i2), 'idf'], w=[bkey])
                    k.cp('act', h2Tf[:, hf * 4:(hf + 1) * 4, :], bank[:, :].rearrange("p (k t) -> p k t", k=4), w=[bkey, ('h2Tf', hf)])
                bi, bank, bkey = nb()
                for kc in range(8):
                    k.mm(bank[:, 0:72], h2Tf[:, kc, :], wrf[:, kc, :], start=(kc == 0), stop=(kc == 7), r=[('h2Tf', 0), ('h2Tf', 1), 'wrf'], w=[bkey])
                k.tt('dve', lgs[:, tt_, :], bank[:, 0:72], bvr[:, :], ALU.add, r=['bvr'], w=[bkey, ('lgs', tt_)])
                if dbg == 'B':
                    k.dma('sp', dbg2_ap[:, gt * 72:(gt + 1) * 72], lgs[:, tt_, :], r=[('lgs', tt_)])
                pass
            NTL = HT // 128
            def RR(t, i, n=1):
                return rs[:, t, i:i + n]
            def LG(t, a, b):
                return lgs[:, t, a:b]
            tiles = list(range(NTL))
            GT = lambda t: half * NTL + t
            RKt = lambda t: [('rs', t)]
            for t in tiles:
                fw.op('dve', lambda e, t=t: e.reduce_max(out=RR(t, 0), in_=LG(t, 0, 8), axis=AX.X), [('lgs', t)], RKt(t))
            for t in tiles:
                k.ts('dve', ohg[:, GT(t), :], LG(t, 0, 8), RR(t, 0), ALU.is_equal, r=[('lgs', t)] + RKt(t), w=[('ohg', GT(t))])
            for t in tiles:
                k.ts('dve', RR(t, 1), RR(t, 0), -1.0, ALU.mult, r=RKt(t), w=RKt(t))
            for t in tiles:
                k.act(RR(t, 8, 8), LG(t, 0, 8), AF.Exp, r=[('lgs', t)] + RKt(t), w=RKt(t), bias=RR(t, 1), accum_out=RR(t, 2))
            for t in tiles:
                k.recip(RR(t, 3), RR(t, 2), r=RKt(t), w=RKt(t))
            for t in tiles:
                k.ts('dve', RR(t, 16, 8), LG(t, 8, 16), ohg[:, GT(t), 0:1], ALU.mult, r=[('lgs', t), ('ohg', GT(t))] + RKt(t), w=RKt(t))
            for g_ in range(1, 8):
                for t in tiles:
                    k.stt(RR(t, 16, 8), LG(t, 8 + 8 * g_, 16 + 8 * g_), ohg[:, GT(t), g_:g_ + 1], RR(t, 16, 8), ALU.mult, ALU.add, r=[('lgs', t), ('ohg', GT(t))] + RKt(t), w=RKt(t))
            for t in tiles:
                fw.op('dve', lambda e, t=t: e.reduce_max(out=RR(t, 4), in_=RR(t, 16, 8), axis=AX.X), RKt(t), RKt(t))
            for t in tiles:
                k.ts('dve', RR(t, 5), RR(t, 4), -1.0, ALU.mult, r=RKt(t), w=RKt(t))
            for t in tiles:
                k.act(RR(t, 24, 8), RR(t, 16, 8), AF.Exp, r=RKt(t), w=RKt(t), bias=RR(t, 5))
            for t in tiles:
                fw.op('dve', lambda e, t=t: e.reduce_max(out=RR(t, 6), in_=RR(t, 24, 8), axis=AX.X), RKt(t), RKt(t))
            for t in tiles:
                k.ts('dve', RR(t, 32, 8), RR(t, 24, 8), RR(t, 6), ALU.is_equal, r=RKt(t), w=RKt(t))
            for t in tiles:
                k.stt(RR(t, 40, 8), RR(t, 32, 8), -4.0, RR(t, 24, 8), ALU.mult, ALU.add, r=RKt(t), w=RKt(t))
            for t in tiles:
                fw.op('dve', lambda e, t=t: e.reduce_max(out=RR(t, 7), in_=RR(t, 40, 8), axis=AX.X), RKt(t), RKt(t))
            for t in tiles:
                k.ts('dve', RR(t, 48, 8), RR(t, 40, 8), RR(t, 7), ALU.is_equal, r=RKt(t), w=RKt(t))
            for t in tiles:
                k.tt('dve', RR(t, 56), RR(t, 6), RR(t, 7), ALU.add, r=RKt(t), w=RKt(t))
            for t in tiles:
                k.recip(RR(t, 57), RR(t, 56), r=RKt(t), w=RKt(t))
            for t in tiles:
                k.tt('dve', RR(t, 57), RR(t, 57), RR(t, 3), ALU.mult, r=RKt(t), w=RKt(t))
            for t in tiles:
                k.tt('dve', RR(t, 58), RR(t, 6), RR(t, 57), ALU.mult, r=RKt(t), w=RKt(t))
            for t in tiles:
                k.tt('dve', RR(t, 59), RR(t, 7), RR(t, 57), ALU.mult, r=RKt(t), w=RKt(t))
            for t in tiles:
                k.ts('dve', RR(t, 32, 8), RR(t, 32, 8), RR(t, 58), ALU.mult, r=RKt(t), w=RKt(t))
            for t in tiles:
                k.stt(RR(t, 32, 8), RR(t, 48, 8), RR(t, 59), RR(t, 32, 8), ALU.mult, ALU.add, r=RKt(t), w=RKt(t))
            for g_ in range(8):
                for t in tiles:
                    k.ts('dve', cwf[:, GT(t), g_ * 8:(g_ + 1) * 8], RR(t, 32, 8), ohg[:, GT(t), g_:g_ + 1], ALU.mult, r=RKt(t) + [('ohg', GT(t))], w=[('cwf', GT(t))])
            fw.flush()
        S_z.close()

    mixer_half(0)
    mixer_half(1)
    if dbg == 'B':
        k.dma('sp', dbg_ap, x1s, r=[])
        fw.flush(final=True)
        S_h2.close(); G.close(); fw.stack.close()
        return nc

    with ExitStack() as C:
        iof = k.sb(C, "iof", [128, GCAP], F32)
        ohb = k.sb(C, "ohb", [128, 16, 8], BF16)
        chi = k.sb(C, "chi", [128, 16, 64], BF16)
        clo = k.sb(C, "clo", [128, 16, 64], BF16)
        pos = k.sb(C, "pos", [128, 16, 8], F32)
        SelG = k.sb(C, "SelG", [128, 16, GCAP], BF16)
        SelGT = k.sb(C, "SelGT", [128, 3, TOK], BF16)
        xgT = k.sb(C, "xgT", [128, 8, GCAP], BF16)
        cws = k.sb(C, "cws", [128, 3, 8], F32)
        yg = k.sb(C, "yg", [128, 3, 1024], F32)
        ygb = k.sb(C, "ygb", [128, 3, 1024], BF16)
        hid = k.sb(C, "hid", [128, 4, GCAP], BF16)
        sgt = [k.sb(C, f"sgt{i}", [128, GCAP], BF16) for i in range(2)]
        stg = [k.sb(C, f"stgc{i}", [128, 1024], F32) for i in range(2)]
        NWB = 3
        wgt = [k.sb(C, f"wgt{i}", [128, 8, 512], BF16) for i in range(NWB)]
        wut = [k.sb(C, f"wut{i}", [128, 8, 512], BF16) for i in range(NWB)]
        wdt = [k.sb(C, f"wdt{i}", [128, 4, 1024], BF16) for i in range(NWB)]
        k.dma('sp', iof[:], consts[:, C_IOTA:C_IOTA + GCAP], w=['iof'])
        OHK = [('ohg', t) for t in range(16)]
        CWK = [('cwf', t) for t in range(16)]
        k.cp('act', ohb[:], ohg[:], r=OHK, w=['ohb'])
        k.cp('act', chi[:], cwf[:], r=CWK, w=['chi'])
        k.tt('dve', clo[:], cwf[:], chi[:], ALU.subtract, r=CWK + ['chi'], w=['clo'])
        for t in range(16):
            bi, bank, bkey = nb()
            k.mm(bank[:, 0:8], lt_b, ohb[:, t, :], start=True, stop=(t == 0), r=['cb', 'ohb'], w=[bkey])
            for t2 in range(t):
                k.mm(bank[:, 0:8], ones_b, ohb[:, t2, :], start=False, stop=(t2 == t - 1), r=['cb', 'ohb'], w=[bkey])
            k.cp('act', pos[:, t, :], bank[:, 0:8], w=[bkey, ('pos', t)])
        wcnt = 0
        H2K = [('h2tm', t) for t in range(16)]

        def load_expert(E):
            i = E % NWB
            k.dma('pool', wgt[i][:], weg[E].rearrange("(k p) f -> p k f", p=128), w=[('wgt', i)])
            k.dma('pool', wut[i][:], weu[E].rearrange("(k p) f -> p k f", p=128), w=[('wut', i)])
            k.dma('pool', wdt[i][:], wed[E].rearrange("(k p) n -> p k n", p=128), w=[('wdt', i)])

        load_expert(0)
        load_expert(1)

        def build_sel(g):
            for t in range(16):
                k.ts('dve', SelG[:, t, :], iof[:, :], pos[:, t, g:g + 1], ALU.is_equal, ohg[:, t, g:g + 1], ALU.mult,
                     r=['iof', ('pos', t), ('ohg', t)], w=[('SelG', t)])

        build_sel(0)
        for g in range(8):
            SK = [('SelG', t) for t in range(16)]
            for kc in range(8):
                bi, bank, bkey = nb()
                for t in range(16):
                    k.mm(bank[:, 0:GCAP], h2tm[:, t, kc * 128:(kc + 1) * 128], SelG[:, t, :], start=(t == 0), stop=(t == 15), r=H2K + SK, w=[bkey])
                k.cp('act' if kc % 2 == 0 else 'dve', xgT[:, kc, :], bank[:, 0:GCAP], w=[bkey, ('xgT', kc)])
            XK = [('xgT', kc) for kc in range(8)]
            for st_ in range(3):
                bi, bank, bkey = nb()
                for t in range(16):
                    k.mm(bank[:, 0:8], SelG[:, t, st_ * 128:(st_ + 1) * 128], chi[:, t, g * 8:(g + 1) * 8], start=(t == 0), stop=False, r=SK + ['chi'], w=[bkey])
                    k.mm(bank[:, 0:8], SelG[:, t, st_ * 128:(st_ + 1) * 128], clo[:, t, g * 8:(g + 1) * 8], start=False, stop=(t == 15), r=SK + ['clo'], w=[bkey])
                k.cp('act', cws[:, st_, :], bank[:, 0:8], w=[bkey, ('cws', st_)])
            for st_ in range(3):
                for t8 in range(2):
                    bi, bank, bkey = nb()
                    psb = bank[:, :].bitcast(BF16)
                    for j in range(8):
                        t = t8 * 8 + j
                        k.tr(psb[:, j * 128:(j + 1) * 128], SelG[:, t, st_ * 128:(st_ + 1) * 128], ident, r=SK + ['cb'], w=[bkey])
                    k.cp('act', SelGT[:, st_, t8 * 1024:(t8 + 1) * 1024], psb[:, 0:1024], w=[bkey, ('SelGT', st_, t8)])
            if g + 1 < 8:
                build_sel(g + 1)
            for e_ in range(8):
                E = g * 8 + e_
                i = E % NWB
                if E + 2 < 64:
                    load_expert(E + 2)
                for fc in range(4):
                    bi, bank, bkey = nb()
                    for kc in range(8):
                        k.mm(bank[:, 0:GCAP], wgt[i][:, kc, fc * 128:(fc + 1) * 128], xgT[:, kc, :], start=(kc == 0), stop=(kc == 7), r=[('wgt', i)] + XK, w=[bkey])
                    sg = sgt[wcnt % 2]
                    k.act(sg[:, :], bank[:, 0:GCAP], AF.Silu, w=[bkey, ('sgt', wcnt % 2)])
                    bi, bank, bkey = nb()
                    for kc in range(8):
                        k.mm(bank[:, 0:GCAP], wut[i][:, kc, fc * 128:(fc + 1) * 128], xgT[:, kc, :], start=(kc == 0), stop=(kc == 7), r=[('wut', i)] + XK, w=[bkey])
                    k.tt('dve', hid[:, fc, :], bank[:, 0:GCAP], sg[:, :], ALU.mult, r=[('sgt', wcnt % 2)], w=[bkey, ('hid', fc)])
                    wcnt += 1
                HK_ = [('hid', fc) for fc in range(4)]
                for st_ in range(3):
                    for nh in range(2):
                        bi, bank, bkey = nb()
                        for fc in range(4):
                            k.mm(bank[:, :], hid[:, fc, st_ * 128:(st_ + 1) * 128], wdt[i][:, fc, nh * 512:(nh + 1) * 512], start=(fc == 0), stop=(fc == 3), r=HK_ + [('wdt', i)], w=[bkey])
                        ysl = yg[:, st_, nh * 512:(nh + 1) * 512]
                        if e_ == 0:
                            k.ts('dve', ysl, bank[:, :], cws[:, st_, e_:e_ + 1], ALU.mult, r=[('cws', st_)], w=[bkey, ('yg', st_, nh)])
                        else:
                            k.stt(ysl, bank[:, :], cws[:, st_, e_:e_ + 1], ysl, ALU.mult, ALU.add, r=[('cws', st_)], w=[bkey, ('yg', st_, nh)])
            YK_ = [('yg', a, b) for a in range(3) for b in range(2)]
            k.cp('act', ygb[:], yg[:], r=YK_, w=['ygb'])
            for t in range(16):
                sgi = t % 2
                for nh in range(2):
                    bi, bank, bkey = nb()
                    for st_ in range(3):
                        k.mm(bank[:, :], SelGT[:, st_, t * 128:(t + 1) * 128], ygb[:, st_, nh * 512:(nh + 1) * 512], start=(st_ == 0), stop=(st_ == 2),
                             r=['ygb'] + [('SelGT', a, b) for a in range(3) for b in range(2)], w=[bkey])
                    k.cp('act' if nh == 0 else 'dve', stg[sgi][:, nh * 512:(nh + 1) * 512], bank[:, :], w=[bkey, ('stg', sgi, nh)])
                fw.op('pool', lambda e, t=t, sgi=sgi: e.dma_start(out=x1s[t * 128:(t + 1) * 128, :], in_=stg[sgi][:], accum_op=ALU.add),
                      [('stg', sgi, 0), ('stg', sgi, 1)], [('x1s', t)], dma=True)
        fw.flush()

    with ExitStack() as D:
        gfb = k.sb(D, "gfb", [128, 1024], F32)
        xo = [k.sb(D, f"xo{i}", [128, 1024], F32) for i in range(2)]
        yo = [k.sb(D, f"yo{i}", [128, 1024], F32) for i in range(2)]
        jk = k.sb(D, "jk", [128, 1024], BF16)
        sd = [k.sb(D, f"sd{i}", [128, 8], F32) for i in range(2)]
        k.dma('sp', gfb[:], bvec[:, BV_GF:BV_GF + 1024], w=['gfb'])
        for t in range(16):
            i = t % 2
            st = sd[i]
            k.dma('sp', xo[i][:], x1s[t * 128:(t + 1) * 128, :], w=[('xo', i)])
            k.act(jk[:], xo[i][:], AF.Square, r=[('xo', i)], w=['jk', ('sd', i, 0)], accum_out=st[:, 0:1])
            k.ts('dve', st[:, 1:2], st[:, 0:1], 1.0 / 1024, ALU.mult, 1e-6, ALU.add, r=[('sd', i, 0)], w=[('sd', i, 1)])
            k.act(st[:, 2:3], st[:, 1:2], AF.Sqrt, r=[('sd', i, 1)], w=[('sd', i, 2)])
            k.recip(st[:, 3:4], st[:, 2:3], r=[('sd', i, 2)], w=[('sd', i, 3)])
            k.stt(yo[i][:], xo[i][:], st[:, 3:4], gfb[:], ALU.mult, ALU.mult, r=[('xo', i), ('sd', i, 3), 'gfb'], w=[('yo', i)])
            k.dma('sp', out[t * 128:(t + 1) * 128, :], yo[i][:], r=[('yo', i)], w=[('out', t)])
        fw.flush(final=True)
    S_h2.close()
    G.close()
    fw.stack.close()
    return nc


def _consts():
    c = np.zeros((128, NCON), np.float32)
    p = np.arange(128)[:, None]
    m = np.arange(128)[None, :]
    same = (p // 64) == (m // 64)
    c[:, C_ID:C_ID + 128] = np.eye(128)
    c[:, C_BO:C_BO + 128] = same
    c[:, C_M3:C_M3 + 128] = same & ((p % 64) < (m % 64))
    c[:, C_M3 + 128:C_M3 + 256] = same & ((p % 64) <= (m % 64))
    c[:, C_M3 + 256:C_M3 + 384] = same & ((p % 64) > (m % 64))
    c[:, C_RS:C_RS + 512] = (np.arange(512)[None, :] % 64 != 0)
    c[:, C_ONE:C_ONE + 128] = 1.0
    c[:, C_LT:C_LT + 128] = p < m
    c[:, C_IOTA:C_IOTA + GCAP] = np.arange(GCAP)[None, :]
    return c


def _fm(v):
    v = np.asarray(v, np.float32).reshape(-1, 128)
    return np.ascontiguousarray(v.T)


def prep_common(inp):
    f = lambda a: np.ascontiguousarray(np.asarray(a, np.float32))
    pv = np.zeros((128, NPV), np.float32)
    pv[:, PV["g_mix"]:PV["g_mix"] + 8] = _fm(inp["g_mix"][0])
    pv[:, PV["g_mem"]:PV["g_mem"] + 8] = _fm(inp["g_mem"][0])
    pv[:, PV["g_ffn"]:PV["g_ffn"] + 8] = _fm(inp["g_ffn"][0])
    for i, nm in enumerate(["mu_w", "mu_a", "mu_g"]):
        pv[:, PV[nm]:PV[nm] + 8] = _fm(inp["mu_wag"][0][i])
    pv[:, PV["b_gate"]:PV["b_gate"] + 24] = _fm(inp["b_gate"][0])
    cw = np.asarray(inp["conv_w"][0], np.float32).reshape(4, 128, 3)
    pv[:, PV["conv_w"]:PV["conv_w"] + 12] = cw.transpose(1, 0, 2).reshape(128, 12)
    for nm, key in [("k_k", "k_k"), ("k_a", "k_a"), ("w0", "w0"), ("a0", "a0"), ("ln_w", "ln_x_w"), ("ln_b", "ln_x_b")]:
        pv[:, PV[nm]:PV[nm] + 4] = _fm(inp[key][0])
    pv[:, PV["r_k"]:PV["r_k"] + 4] = _fm(np.asarray(inp["r_k"][0]).reshape(-1))
    bv = np.zeros((128, NBV), np.float32)
    bv[:, BV_MU:BV_MU + 1536] = np.asarray(inp["mu_rkv"][0], np.float32).reshape(1, 1536)
    bv[:, BV_GF:BV_GF + 1024] = np.asarray(inp["g_final"], np.float32).reshape(1, 1024)
    bv[:, BV_RB:BV_RB + 8] = np.asarray(inp["b_router_group"][0], np.float32).reshape(1, 8)
    bv[:, BV_RB + 8:BV_RB + 72] = np.asarray(inp["b_router_expert"][0], np.float32).reshape(1, 64)
    bv[:, BV_GN:BV_GN + 1024] = np.asarray(inp["g_ffn"][0], np.float32).reshape(1, 1024)
    com = dict(
        w_in=f(inp["w_in"][0]), w_gate=f(inp["w_gate"][0]), w_branch=f(np.asarray(inp["w_branch"][0]).reshape(1536, 1024)),
        w_o=f(inp["w_o"][0]), w_kv=f(inp["w_kv_mem"][0]),
        l1=f(np.concatenate([inp["w_lora1"][0], inp["a_lora1"][0], inp["g_lora1"][0]], axis=1)),
        l2wa=f(np.concatenate([inp["w_lora2"][0], inp["a_lora2"][0]], axis=0)),
        l2g=f(inp["g_lora2"][0]),
        wr=f(np.concatenate([inp["w_router_group"][0], inp["w_router_expert"][0]], axis=1)),
        weg=f(inp["w_exp_gate"][0]), weu=f(inp["w_exp_up"][0]), wed=f(inp["w_exp_down"][0]),
        pvec=pv, bvec=bv, consts=_consts(),
    )
    return com


def prep_core(inp, c, com):
    b, q = c // 4, c % 4
    x = np.asarray(inp["x"], np.float32)
    end = (q + 1) * TOK
    xe = np.zeros((WIN, 1024), np.float32)
    xe[WIN - end:] = x[b, :end]
    d = dict(com)
    d["xext"] = xe
    d["mem"] = np.ascontiguousarray(np.asarray(inp["mem"], np.float32)[b])
    return d


_NC_CACHE = {}


def kernel(**inputs):
    if "nc" not in _NC_CACHE:
        _NC_CACHE["nc"] = build_program()
    nc = _NC_CACHE["nc"]
    com = prep_common(inputs)
    in_maps = [prep_core(inputs, c, com) for c in range(NCORES)]
    res = run_bass_kernel_spmd(nc, in_maps, core_ids=list(range(NCORES)))
    out = np.zeros((2, 8192, 1024), np.float32)
    for c in range(NCORES):
        b, q = c // 4, c % 4
        out[b, q * TOK:(q + 1) * TOK] = res.results[c]["out"]
    return out
```
